# Optimizing a Trainium2 kernel written in Bass

```python
import jax
import jax.numpy as jnp
from jax import lax
import numpy as np

D_MODEL = 1024
BATCH = 8
SEQ = 2048
DEPTH = 1

RET_HEADS = 4
RET_DK = 128
RET_DV = 128
RET_CHUNK = 128
ROPE_BASE = 10000.0
MOBA_HEADS = 8
MOBA_DH = 64
MOBA_BLOCK = 256
MOBA_TOPK = 3
MOBA_QCHUNK = 32
RET_QK_WIDTH = RET_HEADS * RET_DK
RET_V_WIDTH = RET_HEADS * RET_DV
MOBA_WIDTH = MOBA_HEADS * MOBA_DH
MIX_WIDTH = RET_V_WIDTH + MOBA_WIDTH
IN_SPLIT_SIZES = (RET_QK_WIDTH, RET_QK_WIDTH, RET_V_WIDTH, RET_V_WIDTH, MOBA_WIDTH, MOBA_WIDTH, MOBA_WIDTH)
IN_COLS = sum(IN_SPLIT_SIZES)
N_EXPERTS = 256
TOP_K = 8
N_GROUPS = 8
TOPK_GROUPS = 4
EXPERT_FF = 256
SHARED_FF = 256
ROUTED_SCALE = 2.5
MOE_BLOCK = 128
N_MOD = 6
EPS = 1e-6

kernel_name = 'hymba_retnet_moba_moe_adaln'


def rms_norm(x, g=None):
    xf = x.astype(jnp.float32)
    y = xf * lax.rsqrt(jnp.mean(xf * xf, axis=-1, keepdims=True) + EPS)
    if g is not None:
        y = y * g.astype(jnp.float32)
    return y.astype(x.dtype)


def rotary(x, pos):
    half = x.shape[-1] // 2
    inv = ROPE_BASE ** (-jnp.arange(half, dtype=jnp.float32) / half)
    ang = pos[:, None] * inv[None, :]
    cos = jnp.cos(ang)[None, :, None, :].astype(x.dtype)
    sin = jnp.sin(ang)[None, :, None, :].astype(x.dtype)
    x1, x2 = x[..., :half], x[..., half:]
    return jnp.concatenate([x1 * cos - x2 * sin, x1 * sin + x2 * cos], axis=-1)


def swiglu(x, wg, wu, wd):
    return (jax.nn.silu(x @ wg) * (x @ wu)) @ wd


def retention(q, k, v, g):
    B_, S_, H, dk = q.shape
    dv = v.shape[-1]
    C = RET_CHUNK
    N = S_ // C
    dt = q.dtype
    pos = jnp.arange(S_, dtype=jnp.float32)
    q = rotary(q, pos)
    k = rotary(k, pos) * (dk ** -0.5)
    log_g = jnp.log1p(-jnp.exp2(-5.0 - jnp.arange(H, dtype=jnp.float32)))
    idx = jnp.arange(C, dtype=jnp.float32)
    diff = idx[:, None] - idx[None, :]
    decay_in = jnp.where(diff >= 0, jnp.exp(log_g[:, None, None] * jnp.maximum(diff, 0.0)), 0.0)
    q_decay = jnp.exp(log_g[:, None] * (idx + 1.0))
    k_decay = jnp.exp(log_g[:, None] * (C - 1.0 - idx))
    chunk_decay = jnp.exp(log_g * C)
    qc = q.reshape(B_, N, C, H, dk).transpose(0, 3, 1, 2, 4)
    kc = k.reshape(B_, N, C, H, dk).transpose(0, 3, 1, 2, 4)
    vc = v.reshape(B_, N, C, H, dv).transpose(0, 3, 1, 2, 4)
    scores = jnp.einsum('bhncd,bhnmd->bhncm', qc, kc) * decay_in[None, :, None].astype(dt)
    inner = jnp.einsum('bhncm,bhnme->bhnce', scores, vc)
    kv = jnp.einsum('bhnmd,bhnme->nbhde', kc * k_decay[None, :, None, :, None].astype(dt), vc)
    cd = chunk_decay[None, :, None, None].astype(dt)

    def step(state, kv_n):
        return state * cd + kv_n, state

    _, prev = lax.scan(step, jnp.zeros((B_, H, dk, dv), dt), kv)
    cross = jnp.einsum('bhncd,nbhde->bhnce', qc * q_decay[None, :, None, :, None].astype(dt), prev)
    o = (inner + cross).transpose(0, 2, 3, 1, 4).reshape(B_, S_, H, dv)
    o = rms_norm(o)
    return (jax.nn.silu(g) * o).reshape(B_, S_, H * dv)


def moba_attention(q, k, v, q_gain, k_gain):
    B_, S_, H, dh = q.shape
    L = MOBA_BLOCK
    NB = -(-S_ // L)
    Sp = NB * L
    pad = Sp - S_
    q = rms_norm(q, q_gain)
    k = rms_norm(k, k_gain)
    padw = ((0, 0), (0, 0), (0, pad), (0, 0))
    q = jnp.pad(q.transpose(0, 2, 1, 3), padw)
    k = jnp.pad(k.transpose(0, 2, 1, 3), padw)
    v = jnp.pad(v.transpose(0, 2, 1, 3), padw)
    kb = k.reshape(B_, H, NB, L, dh)
    vb = v.reshape(B_, H, NB, L, dh)
    k_mean = jnp.mean(kb.astype(jnp.float32), axis=3).astype(k.dtype)
    gate = jnp.einsum('bhsd,bhnd->bhsn', q, k_mean).astype(jnp.float32)
    q_blk = jnp.arange(Sp) // L
    past = jnp.arange(NB)[None, :] < q_blk[:, None]
    gate = jnp.where(past[None, None], gate, -jnp.inf)
    topk = min(MOBA_TOPK, NB)
    _, sel = lax.top_k(gate, topk)
    sel_valid = jnp.arange(topk)[None, :] < jnp.minimum(q_blk, topk)[:, None]
    scale = dh ** -0.5
    QC = MOBA_QCHUNK
    bi = jnp.arange(B_)[:, None, None, None]
    hi = jnp.arange(H)[None, :, None, None]

    def chunk(ci):
        s0 = ci * QC
        qc = lax.dynamic_slice_in_dim(q, s0, QC, axis=2)
        selc = lax.dynamic_slice_in_dim(sel, s0, QC, axis=2)
        validc = lax.dynamic_slice_in_dim(sel_valid, s0, QC, axis=0)
        own = s0 // L
        k_own = lax.dynamic_index_in_dim(kb, own, axis=2, keepdims=False)
        v_own = lax.dynamic_index_in_dim(vb, own, axis=2, keepdims=False)
        kg = kb[bi, hi, selc]
        vg = vb[bi, hi, selc]
        s_sel = jnp.einsum('bhqd,bhqkld->bhqkl', qc, kg).astype(jnp.float32) * scale
        s_sel = jnp.where(validc[None, None, :, :, None], s_sel, -jnp.inf)
        s_own = jnp.einsum('bhqd,bhld->bhql', qc, k_own).astype(jnp.float32) * scale
        qpos = s0 + jnp.arange(QC)
        kpos = own * L + jnp.arange(L)
        s_own = jnp.where((kpos[None, :] <= qpos[:, None])[None, None], s_own, -jnp.inf)
        logits = jnp.concatenate([s_sel.reshape(B_, H, QC, topk * L), s_own], axis=-1)
        p = jax.nn.softmax(logits, axis=-1).astype(v.dtype)
        p_sel = p[..., :topk * L].reshape(B_, H, QC, topk, L)
        p_own = p[..., topk * L:]
        return (jnp.einsum('bhqkl,bhqkld->bhqd', p_sel, vg)
                + jnp.einsum('bhql,bhld->bhqd', p_own, v_own))

    o = lax.map(chunk, jnp.arange(Sp // QC))
    o = o.transpose(1, 2, 0, 3, 4).reshape(B_, H, Sp, dh)[:, :, :S_]
    return o.transpose(0, 2, 1, 3).reshape(B_, S_, H * dh)


def moe_ffn(h, w_router, router_bias, w_gate, w_up, w_down, ws_gate, ws_up, ws_down):
    B_, S_, D = h.shape
    xf = h.reshape(-1, D)
    N = xf.shape[0]
    scores = jax.nn.sigmoid(jnp.einsum('nd,de->ne', xf, w_router).astype(jnp.float32))
    biased = scores + router_bias.astype(jnp.float32)
    grp = biased.reshape(N, N_GROUPS, N_EXPERTS // N_GROUPS)
    grp_score = jnp.sum(lax.top_k(grp, 2)[0], axis=-1)
    _, top_g = lax.top_k(grp_score, TOPK_GROUPS)
    gmask = jnp.any(top_g[:, :, None] == jnp.arange(N_GROUPS)[None, None, :], axis=1)
    emask = jnp.repeat(gmask, N_EXPERTS // N_GROUPS, axis=1)
    choice = jnp.where(emask, biased, -jnp.inf)
    _, top_e = lax.top_k(choice, TOP_K)
    w = jnp.take_along_axis(scores, top_e, axis=-1)
    w = w / jnp.sum(w, axis=-1, keepdims=True) * ROUTED_SCALE
    A = N * TOP_K
    flat_e = top_e.reshape(A)
    flat_t = jnp.repeat(jnp.arange(N, dtype=jnp.int32), TOP_K)
    flat_w = w.reshape(A)
    order = jnp.argsort(flat_e)
    se = flat_e[order]
    counts = jnp.bincount(flat_e, length=N_EXPERTS)
    padded = (counts + MOE_BLOCK - 1) // MOE_BLOCK * MOE_BLOCK
    pad_end = jnp.cumsum(padded)
    pad_start = pad_end - padded
    start = jnp.cumsum(counts) - counts
    dest = pad_start[se] + jnp.arange(A) - start[se]
    n_blocks = -(-A // MOE_BLOCK) + N_EXPERTS
    P = n_blocks * MOE_BLOCK
    buf_t = jnp.full((P,), N, jnp.int32).at[dest].set(flat_t[order])
    buf_w = jnp.zeros((P,), jnp.float32).at[dest].set(flat_w[order])
    blk_e = jnp.minimum(jnp.searchsorted(pad_end, jnp.arange(n_blocks) * MOE_BLOCK, side='right'), N_EXPERTS - 1)
    x_pad = jnp.concatenate([xf, jnp.zeros((1, D), xf.dtype)], axis=0)

    def expert_block(args):
        tok, wt, e = args
        xb = x_pad[tok]
        hid = jax.nn.silu(xb @ w_gate[e]) * (xb @ w_up[e])
        return (hid @ w_down[e]) * wt[:, None].astype(xb.dtype)

    out = lax.map(expert_block, (buf_t.reshape(n_blocks, MOE_BLOCK), buf_w.reshape(n_blocks, MOE_BLOCK), blk_e))
    routed = jax.ops.segment_sum(out.reshape(P, D), buf_t, num_segments=N + 1)[:N]
    shared = swiglu(xf, ws_gate, ws_up, ws_down)
    return (routed + shared).reshape(B_, S_, D)


def setup_inputs(seed: int = 0) -> dict:
    key = jax.random.key(seed)
    ks = jax.random.split(key, 18)
    D = D_MODEL

    def nrm(k, shape, scale):
        return jax.random.normal(k, shape, jnp.float32) * scale

    return {
        'x': nrm(ks[0], (BATCH, SEQ, D), 1.0),
        'c': nrm(ks[1], (BATCH, D), 1.0),
        'w_ada': nrm(ks[2], (DEPTH, D, N_MOD * D), 0.3 * D ** -0.5),
        'b_ada': nrm(ks[3], (DEPTH, N_MOD * D), 0.02),
        'g_mix': 1.0 + nrm(ks[4], (DEPTH, D), 0.02),
        'w_in': nrm(ks[5], (DEPTH, D, IN_COLS), D ** -0.5),
        'q_gain': 1.0 + nrm(ks[6], (DEPTH, MOBA_DH), 0.02),
        'k_gain': 1.0 + nrm(ks[7], (DEPTH, MOBA_DH), 0.02),
        'w_out': nrm(ks[8], (DEPTH, MIX_WIDTH, D), MIX_WIDTH ** -0.5),
        'g_ffn': 1.0 + nrm(ks[9], (DEPTH, D), 0.02),
        'w_router': nrm(ks[10], (DEPTH, D, N_EXPERTS), D ** -0.5),
        'router_bias': nrm(ks[11], (DEPTH, N_EXPERTS), 0.01),
        'w_gate': nrm(ks[12], (DEPTH, N_EXPERTS, D, EXPERT_FF), D ** -0.5),
        'w_up': nrm(ks[13], (DEPTH, N_EXPERTS, D, EXPERT_FF), D ** -0.5),
        'w_down': nrm(ks[14], (DEPTH, N_EXPERTS, EXPERT_FF, D), EXPERT_FF ** -0.5),
        'ws_gate': nrm(ks[15], (DEPTH, D, SHARED_FF), D ** -0.5),
        'ws_up': nrm(ks[16], (DEPTH, D, SHARED_FF), D ** -0.5),
        'ws_down': nrm(ks[17], (DEPTH, SHARED_FF, D), SHARED_FF ** -0.5),
    }


def reference(x, c, w_ada, b_ada, g_mix, w_in, q_gain, k_gain, w_out, g_ffn, w_router, router_bias,
              w_gate, w_up, w_down, ws_gate, ws_up, ws_down):
    B_, S_, D = x.shape
    split_at = np.cumsum(IN_SPLIT_SIZES)[:-1].tolist()
    for l in range(DEPTH):
        mod = jnp.einsum('bd,de->be', jax.nn.silu(c), w_ada[l]) + b_ada[l]
        sh_a, sc_a, gt_a, sh_f, sc_f, gt_f = [m[:, None, :] for m in jnp.split(mod, N_MOD, axis=-1)]
        h = rms_norm(x, g_mix[l]) * (1.0 + sc_a) + sh_a
        proj = jnp.einsum('bsd,dc->bsc', h, w_in[l])
        rq, rk, rv, rg, mq, mk, mv = jnp.split(proj, split_at, axis=-1)
        ret_out = retention(rq.reshape(B_, S_, RET_HEADS, RET_DK), rk.reshape(B_, S_, RET_HEADS, RET_DK),
                            rv.reshape(B_, S_, RET_HEADS, RET_DV), rg.reshape(B_, S_, RET_HEADS, RET_DV))
        moba_out = moba_attention(mq.reshape(B_, S_, MOBA_HEADS, MOBA_DH), mk.reshape(B_, S_, MOBA_HEADS, MOBA_DH),
                                  mv.reshape(B_, S_, MOBA_HEADS, MOBA_DH), q_gain[l], k_gain[l])
        mixed = jnp.einsum('bsc,cd->bsd', jnp.concatenate([ret_out, moba_out], axis=-1), w_out[l])
        x = x + gt_a * mixed
        h = rms_norm(x, g_ffn[l]) * (1.0 + sc_f) + sh_f
        x = x + gt_f * moe_ffn(h, w_router[l], router_bias[l], w_gate[l], w_up[l], w_down[l],
                               ws_gate[l], ws_up[l], ws_down[l])
    return x
```

```python
import numpy as np
import concourse.bass as bass
import concourse.mybir as mybir
from concourse.bass_utils import run_bass_kernel_spmd

F32 = mybir.dt.float32
BF16 = mybir.dt.bfloat16
I32 = mybir.dt.int32
U32 = mybir.dt.uint32
AF = mybir.ActivationFunctionType
ALU = mybir.AluOpType
AX = mybir.AxisListType

S = 2048
D = 1024
NT = 16
KC = 8
EPS = 1e-6
NEXP = 256
CAP = 256
EFF = 256


class Stream:
    def __init__(self, name):
        self.name = name
        self.items = []
        self.waited = {}


class Prog:
    ENG = ("pe", "act", "dve", "pool", "sp")
    import os as _os
    NO_SELF_WAIT = tuple(_os.environ.get("NO_SELF_WAIT", "pe").split(","))

    def __init__(self, nc):
        self.nc = nc
        self.streams = {n: Stream(n) for n in self.ENG}
        self.sems = {}
        self.semval = {}
        self.last_w = {}
        self.readers = {}
        self.sb_off = 16512
        self.n_ops = 0

    def sb(self, name, shape, dtype):
        esz = 4 if dtype in (F32, I32, U32) else 2
        n = 1
        for s in shape[1:]:
            n *= s
        nbytes = (n * esz + 63) // 64 * 64
        off = self.sb_off
        assert off + nbytes <= 229344, f"SBUF overflow at {name}: {off}+{nbytes}"
        self.sb_off += nbytes
        return self.nc.alloc_sbuf_tensor_at(name, list(shape), dtype, offset=off)

    def mark(self):
        return self.sb_off

    def release(self, m):
        self.sb_off = m

    def _sem(self, key):
        if key not in self.sems:
            self.sems[key] = self.nc.alloc_semaphore("s_" + key)
            self.semval[key] = 0
        return self.sems[key]

    def _wait(self, st, tok):
        if tok is None:
            return
        key, val = tok
        if key == "e_" + st.name and st.name in self.NO_SELF_WAIT:
            return
        if st.waited.get(key, 0) >= val:
            return
        st.waited[key] = val
        st.items.append(("wait", key, val))

    def _wait_all(self, st, deps):
        mx = {}
        for d in deps:
            if d is None:
                continue
            if d[1] > mx.get(d[0], 0):
                mx[d[0]] = d[1]
        for k, v in mx.items():
            self._wait(st, (k, v))

    def wait(self, eng, tok):
        self._wait(self.streams[eng], tok)

    def op(self, eng, fn, reads=(), writes=(), dma=None, sig=True, extra=()):
        st = self.streams[eng]
        deps = list(extra)
        for r in reads:
            deps.append(self.last_w.get(r))
        for w in writes:
            deps.append(self.last_w.get(w))
            deps.extend(self.readers.get(w, ()))
        self._wait_all(st, deps)
        tok = None
        key = None
        inc = 1
        if dma is not None:
            key, inc = dma, 16
        elif sig:
            key = "e_" + eng
        if key is not None:
            self._sem(key)
            self.semval[key] += inc
            tok = (key, self.semval[key])
        st.items.append(("op", fn, key, inc))
        self.n_ops += 1
        if tok is not None:
            for r in reads:
                self.readers.setdefault(r, []).append(tok)
            for w in writes:
                self.last_w[w] = tok
                self.readers[w] = []
        return tok

    def group(self, eng, fns, reads=(), writes=(), extra=()):
        st = self.streams[eng]
        deps = list(extra)
        for r in reads:
            deps.append(self.last_w.get(r))
        for w in writes:
            deps.append(self.last_w.get(w))
            deps.extend(self.readers.get(w, ()))
        self._wait_all(st, deps)
        for fn in fns[:-1]:
            st.items.append(("op", fn, None, 1))
        return self.op(eng, fns[-1], reads=reads, writes=writes)

    def replay(self, eng, e):
        for it in self.streams[eng].items:
            if it[0] == "wait":
                e.wait_ge(self.sems[it[1]], it[2])
            else:
                ins = it[1](e)
                if it[2] is not None:
                    ins.then_inc(self.sems[it[2]], it[3])

    def barrier(self):
        st = self.streams["act"]
        for k, v in self.semval.items():
            if v > 0:
                self._wait(st, (k, v))
        tok = self.op("act", lambda e: e.copy(out=self.bar_scratch[:, 0:1], in_=self.bar_scratch[:, 1:2]))
        for n, s2 in self.streams.items():
            self._wait(s2, tok)
        self.last_w = {}
        self.readers = {}
        return tok

    def reg(self, e, val):
        if not hasattr(self, "_regs"):
            self._regs = {}
        if val not in self._regs:
            self._regs[val] = e.to_reg(val)
        return self._regs[val]

    def final_waits(self, eng, keys):
        st = self.streams[eng]
        for k in keys:
            if k in self.semval and self.semval[k] > 0:
                self._wait(st, (k, self.semval[k]))


def build_nc(debug=(), stop_after=None):
    import os
    import math
    BIS = int(os.environ.get('BIS', '99'))
    nc = bass.Bass("TRN2", target_bir_lowering=False)
    P = Prog(nc)

    def din(name, shape, dt=F32):
        return nc.dram_tensor(name, list(shape), dt, kind="ExternalInput").ap()

    x_d = din("x", [S, D])
    cT_d = din("cT", [128, KC])
    w_ada_d = din("w_ada", [D, 6 * D])
    b_ada_d = din("b_ada", [1, 6 * D])
    gmix_d = din("g_mix_c", [128, KC])
    gffn_d = din("g_ffn_c", [128, KC])
    gffn_r_d = din("g_ffn_r", [1, D])
    w_in_d = din("w_in_x", [D, 4608])
    qg_d = din("q_gain", [1, 64])
    kg_d = din("k_gain", [1, 64])
    w_out_d = din("w_out", [D, D])
    w_router_d = din("w_router", [D, NEXP])
    rbias_d = din("router_bias", [1, NEXP])
    if stop_after is None:
        w_gate_d = din("w_gate", [NEXP, D, EFF])
        w_up_d = din("w_up", [NEXP, D, EFF])
        w_down_d = din("w_down", [NEXP, EFF, D])
    ws_gate_d = din("ws_gate", [D, EFF])
    ws_up_d = din("ws_up", [D, EFF])
    ws_down_d = din("ws_down", [EFF, D])
    y_d = nc.dram_tensor("y", [S, D], F32, kind="ExternalOutput").ap()

    dbg = {}

    def dbg_out(name, shape, dt=F32):
        if name in debug:
            dbg[name] = nc.dram_tensor("dbg_" + name, list(shape), dt, kind="ExternalOutput").ap()
            return dbg[name]
        return None

    h2_d = nc.dram_tensor("h2_scr", [S, D], BF16).ap()
    tokidx_d = nc.dram_tensor("tokidx_scr", [NEXP * CAP, 2], I32).ap()
    o8_d = nc.dram_tensor("o8_scr", [S * 8, D], BF16).ap()

    ps = [nc.alloc_psum_tensor(f"psb{i}", [128, 512], F32) for i in range(8)]

    def psf(b):
        return ps[b][:]

    def psh(b):
        return ps[b][:].bitcast(BF16)

    out_keys = []

    def finish():
        P.final_waits("sp", out_keys)
        with nc.Block() as block:
            @block.tensor
            def _(e):
                P.replay("pe", e)

            @block.scalar
            def _(e):
                P.replay("act", e)

            @block.vector
            def _(e):
                P.replay("dve", e)

            @block.gpsimd
            def _(e):
                P.replay("pool", e)

            @block.sync
            def _(e):
                P.replay("sp", e)
        return nc, P


    ident_bf = P.sb("ident_bf", [128, 128], BF16)
    ident_f = P.sb("ident_f", [128, 128], F32)
    ones_f = P.sb("ones_f", [128, 128], F32)
    ones_bf = P.sb("ones_bf", [128, 128], BF16)
    modT = P.sb("modT", [128, 48], F32)
    A1 = P.sb("A1", [128, KC], F32)
    A2 = P.sb("A2", [128, KC], F32)
    gmix = P.sb("gmix", [128, KC], F32)
    gffn = P.sb("gffn", [128, KC], F32)
    bc = P.sb("bc", [128, 4, D], F32)
    stat = P.sb("stat", [128, 64], F32)
    P.bar_scratch = P.sb("bar_scratch", [128, 2], F32)
    epsb = P.sb("epsb", [128, 1], F32)

    P.op("pool", lambda e: e.memset(ones_f[:], 1.0), writes=["ones_f"])
    P.op("pool", lambda e: e.memset(epsb[:], EPS), writes=["epsb"])
    P.op("pool", lambda e: e.memset(P.bar_scratch[:], 0.0), writes=["bar_scratch"])
    P.op("pool", lambda e: e.memset(ones_bf[:], 1.0), writes=["ones_bf"])
    P.op("pool", lambda e: e.memset(ident_f[:], 0.0), writes=["ident_f"])
    P.op("pool", lambda e: e.affine_select(out=ident_f[:], in_=ones_f[:], pattern=[[-1, 128]],
                                           compare_op=ALU.is_equal, fill=0.0, base=0,
                                           channel_multiplier=1),
         reads=["ones_f"], writes=["ident_f"])
    P.op("dve", lambda e: e.tensor_copy(out=ident_bf[:], in_=ident_f[:]), reads=["ident_f"],
         writes=["ident_bf"])

    T_ = {}
    LNG = [math.log1p(-2.0 ** (-5.0 - h)) for h in range(4)]
    DKS = 128.0 ** -0.5

    def build_tables():
        cosT = P.sb("cosT", [128, S], F32)
        sinT = P.sb("sinT", [128, S], F32)
        mT = P.mark()
        ti = P.sb("ti", [128, S], I32)
        tf = P.sb("tf", [128, S], F32)
        ang = P.sb("ang", [128, S], F32)
        tmpA = P.sb("tmpA", [128, S], F32)
        ji = P.sb("ji", [128, 1], I32)
        jf = P.sb("jf", [128, 1], F32)
        inv = P.sb("inv", [128, 1], F32)
        sgn = P.sb("sgn", [128, 1], F32)
        pi_ = P.sb("pi_", [128, 1], I32)
        pf = P.sb("pf", [128, 1], F32)
        ci = P.sb("ci", [128, 128], I32)
        cf = P.sb("cf", [128, 128], F32)
        biasc = P.sb("biasc", [128, 8], F32)
        PI = math.pi
        for half in range(2):
            P.op("pool", lambda e, half=half: e.iota(out=ji[half * 64:(half + 1) * 64, :], pattern=[[0, 1]], base=0,
                                                    channel_multiplier=1), writes=["ji"])
        P.op("pool", lambda e: e.memset(sgn[0:64, :], -1.0), writes=["sgn"])
        P.op("pool", lambda e: e.memset(sgn[64:128, :], 1.0), writes=["sgn"])
        P.op("dve", lambda e: e.tensor_copy(out=jf[:], in_=ji[:]), reads=["ji"], writes=["jf"])
        P.op("act", lambda e: e.activation(out=inv[:], in_=jf[:], func=AF.Exp, scale=-math.log(10000.0) / 64.0),
             reads=["jf"], writes=["inv"])
        P.op("pool", lambda e: e.iota(out=ti[:], pattern=[[1, S]], base=0, channel_multiplier=0), writes=["ti"])
        P.op("dve", lambda e: e.tensor_copy(out=tf[:], in_=ti[:]), reads=["ti"], writes=["tf"])
        P.op("dve", lambda e: e.tensor_scalar(out=ang[:], in0=tf[:], scalar1=inv[:, 0:1], scalar2=None, op0=ALU.mult),
             reads=["tf", "inv"], writes=["ang"])

        def range_reduce_sin(dst, shift, scale_ap, dname):
            src = "ang"
            if shift != 0.0:
                P.op("pool", lambda e: e.tensor_scalar(out=tmpA[:], in0=ang[:], scalar1=shift, scalar2=None,
                                                       op0=ALU.add), reads=["ang"], writes=["tmpA"])
                a = tmpA
                src = "tmpA"
            else:
                a = ang
            P.op("dve", lambda e: e.tensor_scalar(out=tf[:], in0=a[:], scalar1=1.0 / (2 * PI), scalar2=None,
                                                  op0=ALU.mult), reads=[src], writes=["tf"])
            P.op("dve", lambda e: e.tensor_copy(out=ti[:], in_=tf[:]), reads=["tf"], writes=["ti"])
            P.op("dve", lambda e: e.tensor_copy(out=tf[:], in_=ti[:]), reads=["ti"], writes=["tf"])
            C1 = 6.28125
            C2 = 2 * PI - C1
            P.op("dve", lambda e: e.scalar_tensor_tensor(out=tmpA[:], in0=tf[:], scalar=-C1, in1=a[:], op0=ALU.mult,
                                                         op1=ALU.add), reads=["tf", src], writes=["tmpA"])
            P.op("dve", lambda e: e.scalar_tensor_tensor(out=tmpA[:], in0=tf[:], scalar=-C2, in1=tmpA[:], op0=ALU.mult,
                                                         op1=ALU.add), reads=["tf", "tmpA"], writes=["tmpA"])
            P.op("dve", lambda e: e.tensor_scalar(out=tf[:], in0=tmpA[:], scalar1=PI, scalar2=-2 * PI, op0=ALU.is_gt,
                                                  op1=ALU.mult), reads=["tmpA"], writes=["tf"])
            P.op("pool", lambda e: e.tensor_tensor(out=tmpA[:], in0=tmpA[:], in1=tf[:], op=ALU.add),
                 reads=["tmpA", "tf"], writes=["tmpA"])
            P.op("dve", lambda e: e.tensor_scalar(out=tf[:], in0=tmpA[:], scalar1=-PI, scalar2=2 * PI, op0=ALU.is_lt,
                                                  op1=ALU.mult), reads=["tmpA"], writes=["tf"])
            P.op("pool", lambda e: e.tensor_tensor(out=tmpA[:], in0=tmpA[:], in1=tf[:], op=ALU.add),
                 reads=["tmpA", "tf"], writes=["tmpA"])
            P.op("dve", lambda e: e.tensor_scalar(out=tmpA[:], in0=tmpA[:], scalar1=PI, scalar2=-PI, op0=ALU.min,
                                                  op1=ALU.max), reads=["tmpA"], writes=["tmpA"])
            if scale_ap is None:
                P.op("act", lambda e: e.activation(out=dst[:], in_=tmpA[:], func=AF.Sin), reads=["tmpA"],
                     writes=[dname])
            else:
                P.op("act", lambda e: e.activation(out=dst[:], in_=tmpA[:], func=AF.Sin, scale=scale_ap),
                     reads=["tmpA", "sgn"], writes=[dname])

        range_reduce_sin(sinT, 0.0, sgn[:, 0:1], "sinT")
        range_reduce_sin(cosT, PI / 2, None, "cosT")
        P.op("pool", lambda e: e.iota(out=ci[:], pattern=[[1, 128]], base=1, channel_multiplier=0), writes=["ci"])
        P.op("dve", lambda e: e.tensor_copy(out=cf[:], in_=ci[:]), reads=["ci"], writes=["cf"])
        P.op("pool", lambda e: e.iota(out=pi_[:], pattern=[[0, 1]], base=1, channel_multiplier=1), writes=["pi_"])
        P.op("dve", lambda e: e.tensor_copy(out=pf[:], in_=pi_[:]), reads=["pi_"], writes=["pf"])
        for h in range(4):
            P.op("pool", lambda e, h=h: e.memset(biasc[:, h:h + 1], math.log(DKS)), writes=["biasc"])
            P.op("pool", lambda e, h=h: e.memset(biasc[:, 4 + h:5 + h], 128.0 * LNG[h] + math.log(DKS)),
                 writes=["biasc"])
        for h in range(4):
            P.op("act", lambda e, h=h: e.activation(out=qdec[:, h, :], in_=cf[:], func=AF.Exp, scale=LNG[h]),
                 reads=["cf"], writes=["qdec"])
            P.op("act", lambda e, h=h: e.activation(out=kfac[:, h:h + 1], in_=pf[:], func=AF.Exp, scale=-LNG[h],
                                                    bias=biasc[:, h:h + 1]),
                 reads=["pf", "biasc"], writes=["kfac"])
            P.op("act", lambda e, h=h: e.activation(out=kdec[:, h:h + 1], in_=pf[:], func=AF.Exp, scale=-LNG[h],
                                                    bias=biasc[:, 4 + h:5 + h]),
                 reads=["pf", "biasc"], writes=["kdec"])
        P.op("pool", lambda e: e.affine_select(out=mask01[:], in_=ones_f[:], pattern=[[1, 128]],
                                               compare_op=ALU.is_ge, fill=0.0, base=0, channel_multiplier=-1),
             reads=["ones_f"], writes=["mask01"])

        T_.update(cosT=cosT, sinT=sinT, qdec_done=True, mT=mT, end=P.sb_off)

    if _LAYOUT.get("tbl") is not None:
        _save = P.sb_off
        P.sb_off = _LAYOUT["tbl_small"]
        qdec = P.sb("qdec", [128, 4, 128], F32)
        kfac = P.sb("kfac", [128, 4], F32)
        kdec = P.sb("kdec", [128, 4], F32)
        mask01 = P.sb("mask01", [128, 128], F32)
        assert P.sb_off == _LAYOUT["tbl"]
        _tbl_early = True
        P.sb_off = _save
    else:
        _tbl_early = False

    mA = P.mark()
    cT = P.sb("cT", [128, KC], F32)
    sc = P.sb("sc", [128, KC], F32)
    mod_row = P.sb("mod_row", [1, 6 * D], F32)
    brow = P.sb("brow", [1, 6 * D], F32)
    grow = P.sb("grow", [1, D], F32)
    a2row = P.sb("a2row", [1, D], F32)
    wa = [P.sb(f"wa{i}", [128, KC, 512], BF16) for i in range(4)]
    sc_bf = P.sb("sc_bf", [128, KC], BF16)

    P.op("sp", lambda e: e.dma_start(out=cT[:], in_=cT_d), writes=["cT"], dma="c0")
    P.op("sp", lambda e: e.dma_start(out=brow[:], in_=b_ada_d), writes=["brow"], dma="c1")
    P.op("sp", lambda e: e.dma_start(out=gmix[:], in_=gmix_d), writes=["gmix"], dma="c2")
    P.op("sp", lambda e: e.dma_start(out=gffn[:], in_=gffn_d), writes=["gffn"], dma="c3")
    P.op("sp", lambda e: e.dma_start(out=grow[:], in_=gffn_r_d), writes=["grow"], dma="c4")
    P.op("act", lambda e: e.activation(out=sc[:], in_=cT[:], func=AF.Silu), reads=["cT"], writes=["sc"])
    P.op("act", lambda e: e.copy(out=sc_bf[:], in_=sc[:]), reads=["sc"], writes=["sc_bf"])
    for g in range(12):
        b = g % 4
        P.op("pool", lambda e, g=g, b=b: e.dma_start(
            out=wa[b][:], in_=w_ada_d[:, g * 512:(g + 1) * 512].rearrange("(k p) f -> p k f", p=128)),
            writes=[f"wa{b}"], dma=f"wa{b}")
        fns = []
        for k in range(KC):
            fns.append(lambda e, k=k, b=b: e.matmul(psf(b)[0:1, :], lhsT=sc_bf[:, k:k + 1], rhs=wa[b][:, k, :],
                                                   start=(k == 0), stop=(k == KC - 1)))
        P.group("pe", fns, reads=["sc_bf", f"wa{b}"], writes=[f"ps{b}"])
        P.op("dve", lambda e, g=g, b=b: e.tensor_tensor(out=mod_row[0:1, g * 512:(g + 1) * 512],
                                                        in0=psf(b)[0:1, :], in1=brow[0:1, g * 512:(g + 1) * 512],
                                                        op=ALU.add),
             reads=[f"ps{b}", "brow"], writes=["mod_row"])
        if g == 3 and _tbl_early:
            _save2 = P.sb_off
            P.sb_off = _LAYOUT["tbl"]
            build_tables()
            P.sb_off = _save2
    fns = []
    for j in range(48):
        fns.append(lambda e, j=j: e.matmul(psf(6)[:, j:j + 1], lhsT=mod_row[0:1, j * 128:(j + 1) * 128],
                                           rhs=ones_f[0:1, 0:1], start=True, stop=True))
    P.group("pe", fns, reads=["mod_row", "ones_f"], writes=["ps6"])
    P.op("dve", lambda e: e.tensor_copy(out=modT[:], in_=psf(6)[:, 0:48]), reads=["ps6"], writes=["modT"])
    P.op("dve", lambda e: e.scalar_tensor_tensor(out=A1[:], in0=modT[:, 8:16], scalar=1.0, in1=gmix[:],
                                                 op0=ALU.add, op1=ALU.mult),
         reads=["modT", "gmix"], writes=["A1"])
    P.op("dve", lambda e: e.scalar_tensor_tensor(out=A2[:], in0=modT[:, 32:40], scalar=1.0, in1=gffn[:],
                                                 op0=ALU.add, op1=ALU.mult),
         reads=["modT", "gffn"], writes=["A2"])
    P.op("dve", lambda e: e.scalar_tensor_tensor(out=a2row[:], in0=mod_row[0:1, 4 * D:5 * D], scalar=1.0,
                                                 in1=grow[:], op0=ALU.add, op1=ALU.mult),
         reads=["mod_row", "grow"], writes=["a2row"])
    srcs = [(mod_row, 2 * D), (mod_row, 5 * D), (a2row, 0), (mod_row, 3 * D)]
    for i, (src, off) in enumerate(srcs):
        for hh in range(2):
            b = 4 + (2 * i + hh) % 2
            P.op("pe", lambda e, src=src, off=off, hh=hh, b=b: e.matmul(
                psf(b)[:, :], lhsT=ones_f[0:1, :], rhs=src[0:1, off + hh * 512: off + (hh + 1) * 512],
                start=True, stop=True),
                reads=["mod_row", "a2row", "ones_f"], writes=[f"ps{b}"])
            P.op("act", lambda e, i=i, hh=hh, b=b: e.copy(out=bc[:, i, hh * 512:(hh + 1) * 512], in_=psf(b)[:, :]),
                 reads=[f"ps{b}"], writes=["bc"])
    d = dbg_out("modT", [128, 48])
    if d is not None:
        P.op("sp", lambda e, d=d: e.dma_start(out=d, in_=modT[:]), reads=["modT"], dma="dbg0")
        out_keys.append("dbg0")
    d = dbg_out("bc", [128, 4 * D])
    if d is not None:
        P.op("sp", lambda e, d=d: e.dma_start(out=d, in_=bc[:].rearrange("p a b -> p (a b)")), reads=["bc"], dma="dbg1")
        out_keys.append("dbg1")
    P.barrier()
    P.release(mA)


    if stop_after == "A":
        return finish()

    hT = P.sb("hT", [128, KC, S], BF16)
    cat_off = P.mark()
    concatT = P.sb("concatT", [128, KC, S], BF16)
    cat_end = P.mark()
    ssq = P.sb("ssq", [128, 64], F32)
    rt_ = P.sb("rt_", [128, 64], F32)
    rs_ = P.sb("rs_", [128, 64], F32)
    junk2 = P.sb("junk2", [128, 128], BF16)

    def load_win_group(buf, name, g):
        return P.op("pool", lambda e: e.dma_start(
            out=buf[:], in_=w_in_d[:, g * 512:(g + 1) * 512].rearrange("(k p) f -> p k f", p=128)),
            writes=[name], dma="d_" + name)

    mP1 = P.mark()
    xb = [P.sb(f"xb{i}", [128, D], F32) for i in range(2)]
    xn = [P.sb(f"xn{i}", [128, D], BF16) for i in range(2)]
    junk = P.sb("junk", [128, D], BF16)
    for i in range(NT):
        b = i % 2
        P.op("sp", lambda e, i=i, b=b: e.dma_start(out=xb[b][:], in_=x_d[i * 128:(i + 1) * 128, :]),
             writes=[f"xb{b}"], dma=f"d_xb{b}")
        P.op("act", lambda e, i=i, b=b: e.activation(out=junk[:], in_=xb[b][:], func=AF.Square,
                                                     accum_out=ssq[:, i:i + 1]),
             reads=[f"xb{b}"], writes=["junk", f"ssq{i}"])
        P.op("act", lambda e, i=i: e.activation(out=rt_[:, i:i + 1], in_=ssq[:, i:i + 1], func=AF.Sqrt,
                                                scale=1.0 / D, bias=epsb[:, 0:1]),
             reads=[f"ssq{i}", "epsb"], writes=[f"rt{i}"])
        P.op("dve", lambda e, i=i: e.reciprocal(out=rs_[:, i:i + 1], in_=rt_[:, i:i + 1]),
             reads=[f"rt{i}"], writes=[f"rs{i}"])
        P.op("act", lambda e, i=i, b=b: e.activation(out=xn[b][:], in_=xb[b][:], func=AF.Copy,
                                                     scale=rs_[:, i:i + 1]),
             reads=[f"xb{b}", f"rs{i}"], writes=[f"xn{b}"])
        pb = 5 + b
        fns = [lambda e, c=c, b=b, pb=pb: e.transpose(out=psh(pb)[:, c * 128:(c + 1) * 128],
                                                      in_=xn[b][:, c * 128:(c + 1) * 128], identity=ident_bf[:])
               for c in range(KC)]
        P.group("pe", fns, reads=[f"xn{b}", "ident_bf"], writes=[f"ps{pb}"])
        for c in range(KC):
            if c % 2 == 0:
                P.op("dve", lambda e, i=i, c=c, pb=pb: e.tensor_scalar(
                    out=hT[:, c, i * 128:(i + 1) * 128], in0=psh(pb)[:, c * 128:(c + 1) * 128],
                    scalar1=A1[:, c:c + 1], scalar2=modT[:, c:c + 1], op0=ALU.mult, op1=ALU.add),
                    reads=[f"ps{pb}", "A1", "modT"], writes=[f"hT{i}"])
            else:
                P.op("act", lambda e, i=i, c=c, pb=pb: e.activation(
                    out=hT[:, c, i * 128:(i + 1) * 128], in_=psh(pb)[:, c * 128:(c + 1) * 128],
                    func=AF.Identity, scale=A1[:, c:c + 1], bias=modT[:, c:c + 1]),
                    reads=[f"ps{pb}", "A1", "modT"], writes=[f"hT{i}"])
    P.release(mP1)
    hT_all = [f"hT{i}" for i in range(NT)]
    d = dbg_out("hT", [128, KC * S], BF16)
    if d is not None:
        P.op("sp", lambda e, d=d: e.dma_start(out=d, in_=hT[:].rearrange("p a b -> p (a b)")), reads=hT_all,
             dma="dbg2")
        out_keys.append("dbg2")
    if stop_after == "P1":
        return finish()


    import math
    mR = P.mark()
    LNG = [math.log1p(-2.0 ** (-5.0 - h)) for h in range(4)]
    DKS = 128.0 ** -0.5
    qT = P.sb("qT", [128, 4, S], BF16)
    kT = P.sb("kT", [128, 4, S], BF16)
    v_sb = P.sb("v_sb", [128, NT, 512], BF16)
    sg_sb = P.sb("sg_sb", [128, NT, 512], BF16)
    if _LAYOUT.get("tbl") is None:
        _LAYOUT["tbl_small_new"] = P.sb_off
        qdec = P.sb("qdec", [128, 4, 128], F32)
        kfac = P.sb("kfac", [128, 4], F32)
        kdec = P.sb("kdec", [128, 4], F32)
        mask01 = P.sb("mask01", [128, 128], F32)
    else:
        assert P.sb_off == _LAYOUT["tbl_small"], (P.sb_off, _LAYOUT["tbl_small"])
        P.sb_off = _LAYOUT["tbl"]
    mR2 = P.mark()
    if _LAYOUT.get("tbl") is None:
        _LAYOUT["tbl_new"] = P.sb_off
        build_tables()
    else:
        assert P.sb_off == _LAYOUT["tbl"], (P.sb_off, _LAYOUT["tbl"])
        P.sb_off = T_["end"]
    mT = T_["mT"]
    cosT = T_["cosT"]
    sinT = T_["sinT"]
    P.barrier()
    P.release(mT)

    wi = [P.sb(f"wi{i}", [128, KC, 512], BF16) for i in range(4)]
    rt1 = [P.sb(f"rt1_{i}", [128, 512], F32) for i in range(2)]
    rt2 = [P.sb(f"rt2_{i}", [128, 512], F32) for i in range(2)]
    for gi in range(4):
        load_win_group(wi[gi], f"wi{gi}", gi)
    cnt = 0
    for which in range(2):
        wa_, wp_ = wi[2 * which], wi[2 * which + 1]
        na_, np_ = f"wi{2 * which}", f"wi{2 * which + 1}"
        dstT = qT if which == 0 else kT
        dname = "qT" if which == 0 else "kT"
        for h in range(4):
            for tg in range(4):
                r = cnt % 2
                cnt += 1
                pa, pp = (0, 1) if r == 0 else (2, 3)
                hreads = [f"hT{i}" for i in range(4 * tg, 4 * tg + 4)]
                fns = [lambda e, k=k, h=h, tg=tg, pa=pa, wa_=wa_: e.matmul(
                    psf(pa)[:, :], lhsT=wa_[:, k, h * 128:(h + 1) * 128], rhs=hT[:, k, tg * 512:(tg + 1) * 512],
                    start=(k == 0), stop=(k == KC - 1)) for k in range(KC)]
                P.group("pe", fns, reads=hreads + [na_], writes=[f"ps{pa}"])
                fns = [lambda e, k=k, h=h, tg=tg, pp=pp, wp_=wp_: e.matmul(
                    psf(pp)[:, :], lhsT=wp_[:, k, h * 128:(h + 1) * 128], rhs=hT[:, k, tg * 512:(tg + 1) * 512],
                    start=(k == 0), stop=(k == KC - 1)) for k in range(KC)]
                P.group("pe", fns, reads=hreads + [np_], writes=[f"ps{pp}"])
                P.op("dve", lambda e, r=r, pa=pa, tg=tg: e.tensor_tensor(
                    out=rt1[r][:], in0=psf(pa)[:, :], in1=cosT[:, tg * 512:(tg + 1) * 512], op=ALU.mult),
                    reads=[f"ps{pa}", "cosT"], writes=[f"rt1_{r}"])
                P.op("dve", lambda e, r=r, pp=pp, tg=tg: e.tensor_tensor(
                    out=rt2[r][:], in0=psf(pp)[:, :], in1=sinT[:, tg * 512:(tg + 1) * 512], op=ALU.mult),
                    reads=[f"ps{pp}", "sinT"], writes=[f"rt2_{r}"])
                if which == 0:
                    P.op("pool", lambda e, r=r: e.tensor_tensor(out=rt1[r][:], in0=rt1[r][:], in1=rt2[r][:],
                                                                op=ALU.add),
                         reads=[f"rt1_{r}", f"rt2_{r}"], writes=[f"rt1_{r}"])
                    P.op("pool", lambda e, r=r, h=h, tg=tg: e.tensor_tensor(
                        out=qT[:, h, tg * 512:(tg + 1) * 512].rearrange("p (a b) -> p a b", b=128),
                        in0=rt1[r][:].rearrange("p (a b) -> p a b", b=128),
                        in1=qdec[:, h:h + 1, :].broadcast_to([128, 4, 128]), op=ALU.mult),
                        reads=[f"rt1_{r}", "qdec"], writes=[f"qT{h}_{tg}"])
                else:
                    P.op("pool", lambda e, r=r, h=h, tg=tg: e.tensor_tensor(
                        out=kT[:, h, tg * 512:(tg + 1) * 512], in0=rt1[r][:], in1=rt2[r][:], op=ALU.add),
                        reads=[f"rt1_{r}", f"rt2_{r}"], writes=[f"kT{h}_{tg}"])
    load_win_group(wi[0], "wi0", 4)
    load_win_group(wi[1], "wi1", 5)
    for which in range(2):
        w_ = wi[which]
        for i in range(NT):
            pb = 4 + (i % 2)
            fns = [lambda e, k=k, i=i, pb=pb, w_=w_: e.matmul(
                psf(pb)[:, :], lhsT=hT[:, k, i * 128:(i + 1) * 128], rhs=w_[:, k, :],
                start=(k == 0), stop=(k == KC - 1)) for k in range(KC)]
            P.group("pe", fns, reads=[f"hT{i}", f"wi{which}"], writes=[f"ps{pb}"])
            if which == 0:
                P.op("act", lambda e, i=i, pb=pb: e.copy(out=v_sb[:, i, :], in_=psf(pb)[:, :]),
                     reads=[f"ps{pb}"], writes=[f"v{i}"])
            else:
                P.op("act", lambda e, i=i, pb=pb: e.activation(out=sg_sb[:, i, :], in_=psf(pb)[:, :], func=AF.Silu),
                     reads=[f"ps{pb}"], writes=[f"sg{i}"])
    d = dbg_out("qT", [128, 4 * S], BF16)
    if d is not None:
        P.op("sp", lambda e, d=d: e.dma_start(out=d, in_=qT[:].rearrange("p a b -> p (a b)")),
             reads=[f"qT{h}_{tg}" for h in range(4) for tg in range(4)], dma="dbg3")
        out_keys.append("dbg3")
    d = dbg_out("kT", [128, 4 * S], BF16)
    if d is not None:
        P.op("sp", lambda e, d=d: e.dma_start(out=d, in_=kT[:].rearrange("p a b -> p (a b)")),
             reads=[f"kT{h}_{tg}" for h in range(4) for tg in range(4)], dma="dbg4")
        out_keys.append("dbg4")

    P.barrier()
    P.release(mR2)
    zt = P.sb("zt", [128, 4096], BF16)
    P.op("pool", lambda e: e.memset(zt[:], 0.0), writes=["zt"])
    for zi in range(32):
        P.op("sp", lambda e, zi=zi: e.dma_start(
            out=o8_d[zi * 512:(zi + 1) * 512, :].rearrange("(p a) d -> p (a d)", p=128), in_=zt[:]),
            reads=["zt"], dma="d_o8z")
    o8_init_tok = ("d_o8z", P.semval["d_o8z"])
    S_f = P.sb("S_f", [128, 4, 128], F32)
    S_bf = P.sb("S_bf", [128, 4, 128], BF16)
    ktok = [P.sb(f"ktok{i}", [128, 4, 128], BF16) for i in range(2)]
    PT = [P.sb(f"PT{i}", [128, 4, 128], BF16) for i in range(2)]
    ret_tok = [P.sb(f"ret_tok{i}", [128, 512], BF16) for i in range(2)]
    ssr = P.sb("ssr", [128, NT, 4], F32)
    rtr = P.sb("rtr", [128, NT, 4], F32)
    rsr = P.sb("rsr", [128, NT, 4], F32)
    CD = [math.exp(LNG[h] * 128.0) for h in range(4)]
    def rec_chunk(n):
        tg = n // 4
        r = n % 2
        csl = slice(n * 128, (n + 1) * 128)
        bSC, bO, bKV, bKT = 0 + r, 2 + r, 4 + r, 6 + r
        fns = [lambda e, h=h: e.matmul(psf(bSC)[:, h * 128:(h + 1) * 128], lhsT=kT[:, h, csl], rhs=qT[:, h, csl],
                                       start=True, stop=True) for h in range(4)]
        P.group("pe", fns, reads=[f"kT{h}_{tg}" for h in range(4)] + [f"qT{h}_{tg}" for h in range(4)],
                writes=[f"ps{bSC}"])
        if n < NT - 1:
            fns = [lambda e, h=h: e.transpose(out=psh(bKT)[:, h * 128:(h + 1) * 128], in_=kT[:, h, csl],
                                              identity=ident_bf[:]) for h in range(4)]
            P.group("pe", fns, reads=[f"kT{h}_{tg}" for h in range(4)] + ["ident_bf"], writes=[f"ps{bKT}a"])
        for h in range(4):
            P.op("dve", lambda e, h=h: e.scalar_tensor_tensor(
                out=PT[r][:, h, :], in0=psf(bSC)[:, h * 128:(h + 1) * 128], scalar=kfac[:, h:h + 1], in1=mask01[:],
                op0=ALU.mult, op1=ALU.mult), reads=[f"ps{bSC}", "kfac", "mask01"], writes=[f"PT{r}_{h}"])
        if n < NT - 1:
            for h in range(4):
                P.op("act", lambda e, h=h: e.activation(out=ktok[r][:, h, :], in_=psh(bKT)[:, h * 128:(h + 1) * 128],
                                                        func=AF.Copy, scale=kdec[:, h:h + 1]),
                     reads=[f"ps{bKT}a", "kdec"], writes=[f"ktok{r}_{h}"])
        for h in range(4):
            fns = [lambda e, h=h: e.matmul(psf(bO)[:, h * 128:(h + 1) * 128], lhsT=PT[r][:, h, :],
                                           rhs=v_sb[:, n, h * 128:(h + 1) * 128], start=True, stop=(n == 0))]
            rds = [f"PT{r}_{h}", f"v{n}"]
            if n > 0:
                fns.append(lambda e, h=h: e.matmul(psf(bO)[:, h * 128:(h + 1) * 128], lhsT=qT[:, h, csl],
                                                   rhs=S_bf[:, h, :], start=False, stop=True))
                rds += [f"qT{h}_{tg}", f"S_bf{h}"]
            P.group("pe", fns, reads=rds, writes=[f"ps{bO}"])
        if n < NT - 1:
            for h in range(4):
                P.op("pe", lambda e, h=h: e.matmul(psf(bKV)[:, h * 128:(h + 1) * 128], lhsT=ktok[r][:, h, :],
                                                   rhs=v_sb[:, n, h * 128:(h + 1) * 128], start=True, stop=True),
                     reads=[f"ktok{r}_{h}", f"v{n}"], writes=[f"ps{bKV}"])
            for h in range(4):
                if n == 0:
                    P.op("dve", lambda e, h=h: e.tensor_copy(out=S_f[:, h, :], in_=psf(bKV)[:, h * 128:(h + 1) * 128]),
                         reads=[f"ps{bKV}"], writes=[f"S_f{h}"])
                else:
                    P.op("dve", lambda e, h=h: e.scalar_tensor_tensor(
                        out=S_f[:, h, :], in0=S_f[:, h, :], scalar=CD[h], in1=psf(bKV)[:, h * 128:(h + 1) * 128],
                        op0=ALU.mult, op1=ALU.add), reads=[f"ps{bKV}", f"S_f{h}"], writes=[f"S_f{h}"])
                P.op("pool", lambda e, h=h: e.tensor_copy(out=S_bf[:, h, :], in_=S_f[:, h, :]),
                     reads=[f"S_f{h}"], writes=[f"S_bf{h}"])
        for h in range(4):
            P.op("act", lambda e, h=h: e.activation(out=junk2[:], in_=psf(bO)[:, h * 128:(h + 1) * 128],
                                                    func=AF.Square, accum_out=ssr[:, n, h:h + 1]),
                 reads=[f"ps{bO}"], writes=["junk2", f"ssr{n}"])
        P.op("act", lambda e: e.activation(out=rtr[:, n, :], in_=ssr[:, n, :], func=AF.Sqrt, scale=1.0 / 128.0,
                                           bias=epsb[:, 0:1]), reads=[f"ssr{n}", "epsb"], writes=[f"rtr{n}"])
        P.op("dve", lambda e: e.reciprocal(out=rsr[:, n, :], in_=rtr[:, n, :]), reads=[f"rtr{n}"],
             writes=[f"rsr{n}"])
        for h in range(4):
            P.op("dve", lambda e, h=h: e.scalar_tensor_tensor(
                out=ret_tok[r][:, h * 128:(h + 1) * 128], in0=psf(bO)[:, h * 128:(h + 1) * 128],
                scalar=rsr[:, n, h:h + 1], in1=sg_sb[:, n, h * 128:(h + 1) * 128], op0=ALU.mult, op1=ALU.mult),
                reads=[f"ps{bO}", f"rsr{n}", f"sg{n}"], writes=[f"ret_tok{r}"])
        fns = [lambda e, c=c: e.transpose(out=psh(bKT)[:, 512 + c * 128:512 + (c + 1) * 128],
                                          in_=ret_tok[r][:, c * 128:(c + 1) * 128], identity=ident_bf[:])
               for c in range(4)]
        P.group("pe", fns, reads=[f"ret_tok{r}", "ident_bf"], writes=[f"ps{bKT}b"])
        P.op("act", lambda e: e.copy(out=concatT[:, 0:4, n * 128:(n + 1) * 128],
                                     in_=psh(bKT)[:, 512:1024].rearrange("p (a b) -> p a b", b=128)),
             reads=[f"ps{bKT}b"], writes=[f"cat_r{n}"])

    for n_ in range(NT):
        rec_chunk(n_)
    cat_r = [f"cat_r{n}" for n in range(NT)]
    P.barrier()
    P.release(mR)
    if stop_after == "R":
        d = dbg_out("concatT", [128, KC * S], BF16)
        if d is not None:
            P.op("sp", lambda e, d=d: e.dma_start(out=d, in_=concatT[:].rearrange("p a b -> p (a b)")),
                 dma="dbg5")
            out_keys.append("dbg5")
        return finish()


    mM = P.mark()
    qnT = P.sb("qnT", [128, 4, S], BF16)
    knT = P.sb("knT", [128, 4, S], BF16)
    v_aug = P.sb("v_aug", [128, NT, 8, 128], BF16)
    kmeanT = P.sb("kmeanT", [128, 4, 8], BF16)
    kmtmp = P.sb("kmtmp", [128, 4, 8], F32)
    mask_bf = P.sb("mask_bf", [128, 128], BF16)
    negm = P.sb("negm", [128, 4, 8], F32)
    gain_q = P.sb("gain_q", [128, 64], F32)
    gain_k = P.sb("gain_k", [128, 64], F32)
    mM2 = P.mark()
    wim = [P.sb(f"wim{i}", [128, KC, 512], BF16) for i in range(3)]
    sqj = [P.sb(f"sqj{i}", [128, 512], F32) for i in range(2)]
    nrm1 = [P.sb(f"nrm1_{i}", [128, 512], F32) for i in range(2)]
    qk_tok = [P.sb(f"qk_tok{i}", [128, 512], BF16) for i in range(2)]
    ssm = P.sb("ssm", [128, 2 * NT, 8], F32)
    rtm = P.sb("rtm", [128, 2 * NT, 8], F32)
    rsm = P.sb("rsm", [128, 2 * NT, 8], F32)

    for gi in range(3):
        P.op("pool", lambda e, gi=gi: e.dma_start(
            out=wim[gi][:], in_=w_in_d[:, (6 + gi) * 512:(7 + gi) * 512].rearrange("(k p) f -> p k f", p=128)),
            writes=[f"wim{gi}"], dma=f"d_wim{gi}")
    grow2 = P.sb("grow2", [1, 128], F32)
    P.op("sp", lambda e: e.dma_start(out=grow2[0:1, 0:64], in_=qg_d), writes=["grow2a"], dma="c5")
    P.op("sp", lambda e: e.dma_start(out=grow2[0:1, 64:128], in_=kg_d), writes=["grow2b"], dma="c6")
    P.op("pe", lambda e: e.matmul(psf(7)[:, 0:128], lhsT=ones_f[0:1, :], rhs=grow2[0:1, :], start=True, stop=True),
         reads=["grow2a", "grow2b", "ones_f"], writes=["ps7"])
    P.op("dve", lambda e: e.tensor_copy(out=gain_q[:], in_=psf(7)[:, 0:64]), reads=["ps7"], writes=["gain_q"])
    P.op("dve", lambda e: e.tensor_copy(out=gain_k[:], in_=psf(7)[:, 64:128]), reads=["ps7"], writes=["gain_k"])
    P.op("pool", lambda e: e.memset(v_aug[:].rearrange("p a b c -> p (a b c)"), 1.0), writes=["v_aug"])
    P.op("pool", lambda e: e.affine_select(out=mask_bf[:], in_=ones_f[:], pattern=[[1, 128]],
                                           compare_op=ALU.is_ge, fill=0.0, base=0, channel_multiplier=-1),
         reads=["ones_f"], writes=["mask_bf"])
    P.op("pool", lambda e: e.memset(negm[:].rearrange("p a b -> p (a b)"), 0.0), writes=["negm"])
    for bb in range(4, 8):
        P.op("pool", lambda e, bb=bb: e.memset(negm[:, bb - 4, bb:8], -1.0e30), writes=["negm"])

    def m_mm(i):
        for which in range(3):
            pb = which + 3 * (i % 2)
            fns = [lambda e, k=k, pb=pb, which=which: e.matmul(
                psf(pb)[:, :], lhsT=hT[:, k, i * 128:(i + 1) * 128], rhs=wim[which][:, k, :],
                start=(k == 0), stop=(k == KC - 1)) for k in range(KC)]
            P.group("pe", fns, reads=[f"hT{i}", f"wim{which}"], writes=[f"ps{pb}"])

    def m_post_one(i, which):
        pb = which + 3 * (i % 2)
        if which == 2:
            P.op("act", lambda e: e.copy(
                out=v_aug[:, i, 0::2, 0:64],
                in_=psf(pb)[:, :].rearrange("p (a u d) -> p a u d", u=2, d=64)[:, :, 0, :]),
                reads=[f"ps{pb}"], writes=[f"va{i}"], extra=[v_aug_tok])
            P.op("act", lambda e: e.copy(
                out=v_aug[:, i, 1::2, 64:128],
                in_=psf(pb)[:, :].rearrange("p (a u d) -> p a u d", u=2, d=64)[:, :, 1, :]),
                reads=[f"ps{pb}"], writes=[f"va{i}"], extra=[v_aug_tok])
            return
        col = 2 * i + which
        r = which
        gain = gain_q if which == 0 else gain_k
        gname = "gain_q" if which == 0 else "gain_k"
        P.op("act", lambda e: e.activation(out=sqj[r][:], in_=psf(pb)[:, :], func=AF.Square),
             reads=[f"ps{pb}"], writes=[f"sqj{r}"])
        P.op("dve", lambda e: e.tensor_reduce(out=ssm[:, col, :], in_=sqj[r][:].rearrange("p (h d) -> p h d", d=64),
                                              axis=AX.X, op=ALU.add),
             reads=[f"sqj{r}"], writes=[f"ssm{col}"])
        P.op("act", lambda e: e.activation(out=rtm[:, col, :], in_=ssm[:, col, :], func=AF.Sqrt,
                                           scale=1.0 / 64.0, bias=epsb[:, 0:1]),
             reads=[f"ssm{col}", "epsb"], writes=[f"rtm{col}"])
        P.op("dve", lambda e: e.reciprocal(out=rsm[:, col, :], in_=rtm[:, col, :]),
             reads=[f"rtm{col}"], writes=[f"rsm{col}"])
        P.op("dve", lambda e: e.tensor_tensor(
            out=nrm1[r][:].rearrange("p (h d) -> p h d", d=64),
            in0=psf(pb)[:, :].rearrange("p (h d) -> p h d", d=64),
            in1=rsm[:, col, :].unsqueeze(2).broadcast_to([128, 8, 64]), op=ALU.mult),
            reads=[f"ps{pb}", f"rsm{col}"], writes=[f"nrm1_{r}"])
        P.op("pool", lambda e: e.tensor_tensor(
            out=qk_tok[r][:].rearrange("p (h d) -> p h d", d=64),
            in0=nrm1[r][:].rearrange("p (h d) -> p h d", d=64),
            in1=gain[:].unsqueeze(1).broadcast_to([128, 8, 64]), op=ALU.mult),
            reads=[f"nrm1_{r}", gname], writes=[f"qk_tok{r}"])
        to = which * 512
        fns = [lambda e, c=c: e.transpose(out=psh(6)[:, to + c * 128:to + (c + 1) * 128],
                                          in_=qk_tok[r][:, c * 128:(c + 1) * 128], identity=ident_bf[:])
               for c in range(4)]
        P.group("pe", fns, reads=[f"qk_tok{r}", "ident_bf"], writes=[f"ps6_{which}"])
        dstT = qnT if which == 0 else knT
        P.op("act", lambda e: e.copy(out=dstT[:, :, i * 128:(i + 1) * 128],
                                     in_=psh(6)[:, to:to + 512].rearrange("p (a b) -> p a b", b=128)),
             reads=[f"ps6_{which}"], writes=[("qn" if which == 0 else "kn") + str(i)])
        if which == 1:
            fns = [lambda e, c=c: e.matmul(psf(7)[:, c * 16 + i:c * 16 + i + 1],
                                           lhsT=qk_tok[r][:, c * 128:(c + 1) * 128], rhs=ones_bf[:, 0:1],
                                           start=True, stop=True) for c in range(4)]
            P.group("pe", fns, reads=[f"qk_tok{r}", "ones_bf"], writes=["ps7"])

    v_aug_tok = P.last_w.get("v_aug")
    m_mm(0)
    for i in range(NT):
        if i + 1 < NT:
            m_mm(i + 1)
        for which in range(3):
            m_post_one(i, which)
    P.op("dve", lambda e: e.tensor_tensor(
        out=kmtmp[:], in0=psf(7)[:, 0:64].rearrange("p (c n t) -> p c n t", c=4, t=2)[:, :, :, 0],
        in1=ones_f[:, 0:32].rearrange("p (c n) -> p c n", c=4), op=ALU.mult),
        reads=["ps7", "ones_f"], writes=["kmtmp"])
    P.op("dve", lambda e: e.tensor_tensor(
        out=kmtmp[:], in0=psf(7)[:, 0:64].rearrange("p (c n t) -> p c n t", c=4, t=2)[:, :, :, 1],
        in1=kmtmp[:], op=ALU.add),
        reads=["ps7", "kmtmp"], writes=["kmtmp"])
    P.op("dve", lambda e: e.tensor_scalar(out=kmeanT[:], in0=kmtmp[:], scalar1=1.0 / 256.0, scalar2=None,
                                          op0=ALU.mult), reads=["kmtmp"], writes=["kmeanT"])
    P.barrier()
    if stop_after == "M1":
        d = dbg_out("qnT", [128, 4 * S], BF16)
        if d is not None:
            P.op("sp", lambda e, d=d: e.dma_start(out=d, in_=qnT[:].rearrange("p a b -> p (a b)")), dma="dbg6")
            out_keys.append("dbg6")
        return finish()
    P.release(mM2)
    gm = P.sb("gm", [128, 8, 8], F32)
    m8 = P.sb("m8", [128, 8, 8], F32)
    bsel = P.sb("bsel", [128, 8, 8], F32)
    bsel_bf = P.sb("bsel_bf", [128, 2, 64], BF16)
    pexp = [P.sb(f"pexp{i}", [128, 256], BF16) for i in range(4)]
    rden = [P.sb(f"rden{i}", [128, 256], F32) for i in range(2)]
    biasT8s = [P.sb(f"biasT8_{i}", [128, 256], BF16) for i in range(2)]
    ind64 = P.sb("ind64", [128, 64, 128], BF16)
    P.op("pool", lambda e: e.memset(ind64[:].rearrange("p a b -> p (a b)"), 1.0), writes=["ind64"])
    for hf in range(2):
        P.op("pool", lambda e, hf=hf: e.affine_select(
            out=ind64[hf * 64:(hf + 1) * 64].rearrange("p a b -> p (a b)"),
            in_=ind64[hf * 64:(hf + 1) * 64].rearrange("p a b -> p (a b)"),
            pattern=[[-1, 64], [0, 128]], compare_op=ALU.is_equal, fill=0.0, base=0, channel_multiplier=1),
            reads=["ind64"], writes=["ind64"])

    def emit_bias(i):
        bb = i // 2
        biasT8 = biasT8s[bb % 2]
        bname = f"biasT8_{bb % 2}"
        io = (i % 2) * 128
        fns = [lambda e, h=h, i=i: e.matmul(
            psf(4 if h % 2 == 0 else 7)[:, (h // 2) * 8:(h // 2 + 1) * 8],
            lhsT=qnT[(h % 2) * 64:(h % 2 + 1) * 64, h // 2, i * 128:(i + 1) * 128],
            rhs=kmeanT[(h % 2) * 64:(h % 2 + 1) * 64, h // 2, :], start=True, stop=True) for h in range(8)]
        P.group("pe", fns, writes=["ps4", "ps7"])
        if BIS < 2:
            return
        for par, bk in ((0, 4), (1, 7)):
            P.op("dve", lambda e, bb=bb, par=par, bk=bk: e.tensor_tensor(
                out=gm[:, par::2, :], in0=psf(bk)[:, 0:32].rearrange("p (h n) -> p h n", n=8),
                in1=negm[:, bb - 4:bb - 3, :].broadcast_to([128, 4, 8]), op=ALU.add),
                reads=[f"ps{bk}"], writes=["gm"])
        if BIS < 3:
            return
        for h in range(8):
            P.op("dve", lambda e, h=h: e.max(out=m8[:, h, :], in_=gm[:, h, :]), reads=["gm"], writes=["m8"])
        if BIS < 4:
            return
        P.op("dve", lambda e: e.tensor_tensor(out=bsel[:], in0=gm[:], in1=m8[:, :, 2:3].broadcast_to([128, 8, 8]),
                                              op=ALU.is_lt), reads=["gm", "m8"], writes=["bsel"])
        if BIS < 5:
            return
        for dup in range(2):
            P.op("dve", lambda e, dup=dup: e.tensor_scalar(
                out=bsel_bf[:, dup, :], in0=bsel[:].rearrange("p a b -> p (a b)"), scalar1=-30000.0, scalar2=None,
                op0=ALU.mult), reads=["bsel"], writes=["bsel_bf"])
        if BIS < 6:
            return
        P.op("pe", lambda e: e.transpose(out=psh(4)[:, 512:640], in_=bsel_bf[:].rearrange("p a b -> p (a b)"),
                                         identity=ident_bf[:]), reads=["bsel_bf", "ident_bf"], writes=["ps4"])
        if BIS < 7:
            return
        P.op("act", lambda e, io=io, biasT8=biasT8: e.copy(out=biasT8[:, io:io + 128], in_=psh(4)[:, 512:640]),
             reads=["ps4"], writes=[bname])

    if stop_after == "M2":
        emit_bias(8)
        emit_bias(9)
        P.barrier()
        return finish()
    lnd = [P.sb(f"lnd{i}", [128, 256], F32) for i in range(2)]
    tasks = []
    for bb in range(8 if stop_after != "M3" else 1):
        for j in range(4):
            for u in range(2):
                chunks = []
                for n in range(bb):
                    chunks.append((n, 2 * n, "past"))
                    chunks.append((n, 2 * n + 1, "past"))
                chunks.append((bb, 2 * bb, "own0"))
                chunks.append((bb, 2 * bb + 1, "own1"))
                for ci_, (n, kt, kind) in enumerate(chunks):
                    tasks.append(dict(bb=bb, j=j, u=u, h=2 * j + u, n=n, kt=kt, kind=kind, ci=ci_,
                                      last=(ci_ == len(chunks) - 1), first_of_block=(j == 0 and u == 0 and ci_ == 0)))
    SCB = [0, 1, 6, 5]
    LA = 3
    ocnt_map = {}
    oc = 0
    for t_ in tasks:
        key_ = (t_["bb"], t_["h"])
        if key_ not in ocnt_map:
            ocnt_map[key_] = oc
            oc += 1

    def emit_qk(tix, tk):
        bb, j, u, h, n, kt, kind = tk["bb"], tk["j"], tk["u"], tk["h"], tk["n"], tk["kt"], tk["kind"]
        if tk["first_of_block"] and bb >= 4:
            emit_bias(2 * bb)
            emit_bias(2 * bb + 1)
        biasT8 = biasT8s[bb % 2]
        bname = f"biasT8_{bb % 2}"
        pr = slice(u * 64, (u + 1) * 64)
        q0 = bb * 256
        sb_ = SCB[tix % 4]
        ks = slice(kt * 128, (kt + 1) * 128)
        if kind == "own1":
            qs = slice(q0 + 128, q0 + 256)
            bs = slice(128, 256)
            nq = 128
        else:
            qs = slice(q0, q0 + 256)
            bs = slice(0, 256)
            nq = 256
        use_bias = (kind == "past" and bb >= 4)
        fns = [lambda e: e.matmul(psf(sb_)[:, 0:nq], lhsT=knT[pr, j, ks], rhs=qnT[pr, j, qs], start=True,
                                  stop=(not use_bias))]
        rds = []
        if use_bias:
            fns.append(lambda e: e.matmul(psf(sb_)[:, 0:nq], lhsT=ind64[pr, h * 8 + n, :], rhs=biasT8[pr, bs],
                                          start=False, stop=True))
            rds = [bname, "ind64"]
        P.group("pe", fns, reads=rds, writes=[f"ps{sb_}"])

    def emit_pv(tix, tk):
        bb, j, u, h, n, kt, kind = tk["bb"], tk["j"], tk["u"], tk["h"], tk["n"], tk["kt"], tk["kind"]
        pr = slice(u * 64, (u + 1) * 64)
        dr = slice((1 - u) * 64, (2 - u) * 64)
        q0 = bb * 256
        sb_ = SCB[tix % 4]
        pxb = tix % 4
        oc_ = ocnt_map[(bb, h)]
        ob = 2 + (oc_ % 2)
        nq = 128 if kind == "own1" else 256
        P.op("act", lambda e: e.activation(out=pexp[pxb][:, 0:nq], in_=psf(sb_)[:, 0:nq], func=AF.Exp, scale=0.125),
             reads=[f"ps{sb_}"], writes=[f"pexp{pxb}"])
        if kind in ("own0", "own1"):
            P.op("pool", lambda e: e.tensor_tensor(out=pexp[pxb][:, 0:128], in0=pexp[pxb][:, 0:128], in1=mask_bf[:],
                                                   op=ALU.mult),
                 reads=[f"pexp{pxb}", "mask_bf"], writes=[f"pexp{pxb}"])
        oreg = psf(ob)[:, 128:256] if kind == "own1" else psf(ob)[:, 0:256]
        P.op("pe", lambda e: e.matmul(oreg, lhsT=v_aug[:, kt, h, :], rhs=pexp[pxb][:, 0:nq], start=(tk["ci"] == 0),
                                      stop=tk["last"]),
             reads=[f"pexp{pxb}", f"va{kt}"], writes=[f"ps{ob}"])
        if tk["last"]:
            rb_ = oc_ % 2
            P.op("act", lambda e: e.activation(out=lnd[rb_][pr, :], in_=psf(ob)[dr, 0:256], func=AF.Ln),
                 reads=[f"ps{ob}"], writes=[f"lnd{rb_}"])
            P.op("act", lambda e: e.activation(out=rden[rb_][pr, :], in_=lnd[rb_][pr, :], func=AF.Exp, scale=-1.0),
                 reads=[f"lnd{rb_}"], writes=[f"rden{rb_}"])
            P.op("dve", lambda e: e.tensor_tensor(out=concatT[pr, 4 + j, q0:q0 + 256], in0=psf(ob)[pr, 0:256],
                                                  in1=rden[rb_][pr, :], op=ALU.mult),
                 reads=[f"ps{ob}", f"rden{rb_}"], writes=[f"cat_m{h}_{bb}"])

    for tix in range(len(tasks) + LA):
        if tix < len(tasks):
            emit_qk(tix, tasks[tix])
        if tix - LA >= 0:
            emit_pv(tix - LA, tasks[tix - LA])
    P.barrier()
    P.release(mM)
    if stop_after == "M":
        d = dbg_out("concatT", [128, KC * S], BF16)
        if d is not None:
            P.op("sp", lambda e, d=d: e.dma_start(out=d, in_=concatT[:].rearrange("p a b -> p (a b)")),
                 dma="dbg5")
            out_keys.append("dbg5")
        return finish()


    xres = P.sb("xres", [128, NT, D], F32)
    mO = P.mark()
    wo = P.sb("wo", [128, KC, D], BF16)
    P.op("pool", lambda e: e.dma_start(out=wo[:], in_=w_out_d.rearrange("(k p) f -> p k f", p=128)),
         writes=["wo"], dma="d_wo")
    for i in range(NT):
        P.op("sp", lambda e, i=i: e.dma_start(out=xres[:, i, :], in_=x_d[i * 128:(i + 1) * 128, :]),
             writes=[f"xres{i}"], dma="d_xres")
    for i in range(NT):
        P.last_w[f"xres{i}"] = ("d_xres", P.semval["d_xres"])
    for k in range(KC):
        eng = "dve" if k % 2 == 0 else "pool"
        P.op(eng, lambda e, k=k: e.tensor_tensor(out=wo[:, k, :], in0=wo[:, k, :], in1=bc[:, 0, :], op=ALU.mult),
             reads=["wo", "bc"], writes=["wo"])
    for i in range(NT):
        for hh in range(2):
            pb = (2 * i + hh) % 4
            fns = [lambda e, c=c, i=i, hh=hh, pb=pb: e.matmul(
                psf(pb)[:, :], lhsT=concatT[:, c, i * 128:(i + 1) * 128], rhs=wo[:, c, hh * 512:(hh + 1) * 512],
                start=(c == 0), stop=(c == KC - 1)) for c in range(KC)]
            P.group("pe", fns, reads=["wo"], writes=[f"ps{pb}"])
            P.op("dve", lambda e, i=i, hh=hh, pb=pb: e.tensor_tensor(
                out=xres[:, i, hh * 512:(hh + 1) * 512], in0=psf(pb)[:, :], in1=xres[:, i, hh * 512:(hh + 1) * 512],
                op=ALU.add), reads=[f"ps{pb}", f"xres{i}"], writes=[f"xres{i}"])
    d = dbg_out("x1", [S, D])
    if d is not None:
        for i in range(NT):
            P.op("sp", lambda e, d=d, i=i: e.dma_start(out=d[i * 128:(i + 1) * 128, :], in_=xres[:, i, :]),
                 reads=[f"xres{i}"], dma="dbg7")
        out_keys.append("dbg7")
    P.barrier()
    P.release(mO)
    if stop_after == "O":
        return finish()

    dbg_out("Wd", [S, NEXP])
    Mall = P.sb("Mall", [128, NT, NEXP], BF16)
    dest8 = P.sb("dest8", [128, NT, 8], I32)
    W8 = P.sb("W8", [128, NT, 8], F32)
    mF = P.mark()
    wr = P.sb("wr", [128, KC, NEXP], F32)
    rb_row = P.sb("rb_row", [1, NEXP], F32)
    rb_bc = P.sb("rb_bc", [128, NEXP], F32)
    Ltri = P.sb("Ltri", [128, 128], BF16)
    base_e = P.sb("base_e", [128, NEXP], F32)
    base_i = P.sb("base_i", [128, NEXP], I32)
    cnt_bc = P.sb("cnt_bc", [128, NEXP], F32)
    junkF = P.sb("junkF", [128, D], BF16)
    h2f = [P.sb(f"h2f{i}", [128, D], F32) for i in range(2)]
    h2b = [P.sb(f"h2b{i}", [128, D], BF16) for i in range(2)]
    h2fT = P.sb("h2fT", [128, KC, 128], F32)
    sc_t2 = [P.sb(f"sc_t{i}", [128, NEXP], F32) for i in range(2)]
    biased = P.sb("biased", [128, NEXP], F32)
    choice = P.sb("choice", [128, NEXP], F32)
    Mf = P.sb("Mf", [128, NEXP], F32)
    ws_t = P.sb("ws_t", [128, NEXP], F32)
    Wd = P.sb("Wd", [128, NEXP], F32)
    keyt = P.sb("keyt", [128, NEXP], F32)
    jk = P.sb("jk", [128, NEXP], F32)
    g8 = P.sb("g8", [128, 8, 8], F32)
    gs = P.sb("gs", [128, 8], F32)
    gs8 = P.sb("gs8", [128, 8], F32)
    pen = P.sb("pen", [128, 8], F32)
    t8 = P.sb("t8", [128, 8], F32)
    key8 = P.sb("key8", [128, 8], F32)
    sm = P.sb("sm", [128, 4 * NT], F32)

    P.op("sp", lambda e: e.dma_start(out=wr[:], in_=w_router_d.rearrange("(k p) f -> p k f", p=128)),
         writes=["wr"], dma="c7")
    P.op("sp", lambda e: e.dma_start(out=rb_row[:], in_=rbias_d), writes=["rb_row"], dma="c8")
    P.op("pe", lambda e: e.matmul(psf(7)[:, 0:NEXP], lhsT=ones_f[0:1, :], rhs=rb_row[0:1, :], start=True, stop=True),
         reads=["rb_row", "ones_f"], writes=["ps7"])
    P.op("dve", lambda e: e.tensor_copy(out=rb_bc[:], in_=psf(7)[:, 0:NEXP]), reads=["ps7"], writes=["rb_bc"])
    P.op("pool", lambda e: e.affine_select(out=Ltri[:], in_=ones_f[:], pattern=[[1, 128]], compare_op=ALU.is_gt,
                                           fill=0.0, base=0, channel_multiplier=-1),
         reads=["ones_f"], writes=["Ltri"])
    P.op("pool", lambda e: e.iota(out=base_i[:], pattern=[[CAP, NEXP]], base=1, channel_multiplier=0),
         writes=["base_i"])
    P.op("dve", lambda e: e.tensor_copy(out=base_e[:], in_=base_i[:]), reads=["base_i"], writes=["base_e"])
    P.op("pool", lambda e: e.memset(cnt_bc[:], 0.0), writes=["cnt_bc"])
    bigi = P.sb("bigi", [128, 1024], I32)
    tokid = P.sb("tokid", [128, NT, 8, 2], I32)
    P.op("pool", lambda e: e.iota(out=bigi[:], pattern=[[0, 1024]], base=1 << 20, channel_multiplier=0),
         writes=["bigi"])
    tok_init = P.op("sp", lambda e: e.dma_start(out=tokidx_d.rearrange("(p f) o -> p (f o)", p=128), in_=bigi[:]),
                    reads=["bigi"], dma="c12")
    P.op("pool", lambda e: e.iota(out=tokid[:, :, :, 0], pattern=[[128, NT], [0, 8]], base=0,
                                  channel_multiplier=1), writes=["tokid"])
    P.op("pool", lambda e: e.iota(out=tokid[:, :, :, 1], pattern=[[1024, NT], [1, 8]], base=0,
                                  channel_multiplier=8), writes=["tokid"])

    def f_stageA(i):
        b = i % 2
        xr = f"xres{i}"
        P.op("act", lambda e, i=i: e.activation(out=junkF[:], in_=xres[:, i, :], func=AF.Square,
                                                accum_out=sm[:, 4 * i:4 * i + 1]),
             reads=[xr], writes=["junkF", f"sm{i}a"])
        P.op("act", lambda e, i=i: e.activation(out=sm[:, 4 * i + 1:4 * i + 2], in_=sm[:, 4 * i:4 * i + 1],
                                                func=AF.Sqrt, scale=1.0 / D, bias=epsb[:, 0:1]),
             reads=[f"sm{i}a", "epsb"], writes=[f"sm{i}b"])
        P.op("dve", lambda e, i=i: e.reciprocal(out=sm[:, 4 * i + 2:4 * i + 3], in_=sm[:, 4 * i + 1:4 * i + 2]),
             reads=[f"sm{i}b"], writes=[f"sm{i}c"])
        P.op("dve", lambda e, i=i, b=b: e.scalar_tensor_tensor(
            out=h2f[b][:], in0=xres[:, i, :], scalar=sm[:, 4 * i + 2:4 * i + 3], in1=bc[:, 2, :], op0=ALU.mult,
            op1=ALU.mult), reads=[xr, f"sm{i}c", "bc"], writes=[f"h2f{b}"])
        P.op("pool", lambda e, b=b: e.tensor_tensor(out=h2f[b][:], in0=h2f[b][:], in1=bc[:, 3, :], op=ALU.add),
             reads=[f"h2f{b}", "bc"], writes=[f"h2f{b}"])
        P.op("act", lambda e, b=b: e.copy(out=h2b[b][:], in_=h2f[b][:]), reads=[f"h2f{b}"], writes=[f"h2b{b}"])
        fns = [lambda e, c=c, b=b: e.transpose(out=psh(0)[:, c * 128:(c + 1) * 128],
                                               in_=h2b[b][:, c * 128:(c + 1) * 128], identity=ident_bf[:])
               for c in range(KC)]
        P.group("pe", fns, reads=[f"h2b{b}", "ident_bf"], writes=["ps0"])
        P.op("act", lambda e, i=i: e.copy(out=hT[:, :, i * 128:(i + 1) * 128],
                                          in_=psh(0)[:, :].rearrange("p (a b) -> p a b", b=128)),
             reads=["ps0"], writes=[f"hT{i}"])
        for half in range(2):
            fns = [lambda e, c=c, b=b, half=half: e.transpose(
                out=psf(1 + half)[:, (c % 4) * 128:(c % 4 + 1) * 128], in_=h2f[b][:, c * 128:(c + 1) * 128],
                identity=ident_f[:]) for c in range(4 * half, 4 * half + 4)]
            P.group("pe", fns, reads=[f"h2f{b}", "ident_f"], writes=[f"ps{1 + half}"])
            P.op("act", lambda e, half=half: e.copy(
                out=h2fT[:, 4 * half:4 * half + 4, :],
                in_=psf(1 + half)[:, :].rearrange("p (a b) -> p a b", b=128)),
                reads=[f"ps{1 + half}"], writes=[f"h2fT{half}"])
        fns = [lambda e, c=c: e.matmul(psf(3)[:, 0:NEXP], lhsT=h2fT[:, c, :], rhs=wr[:, c, :], start=(c == 0),
                                       stop=(c == KC - 1)) for c in range(KC)]
        P.group("pe", fns, reads=["h2fT0", "h2fT1", "wr"], writes=["ps3"])
        P.op("act", lambda e: e.activation(out=sc_t2[i % 2][:], in_=psf(3)[:, 0:NEXP], func=AF.Sigmoid), reads=["ps3"],
             writes=[f"sc_t{i % 2}"])

    def f_stageB(i):
        b = i % 2
        P.op("dve", lambda e: e.tensor_tensor(out=biased[:], in0=sc_t2[i % 2][:], in1=rb_bc[:], op=ALU.add),
             reads=[f"sc_t{i % 2}", "rb_bc"], writes=["biased"])
        for g in range(8):
            P.op("dve", lambda e, g=g: e.max(out=g8[:, g, :], in_=biased[:, g * 32:(g + 1) * 32]),
                 reads=["biased"], writes=["g8"])
        P.op("dve", lambda e: e.tensor_tensor(out=gs[:], in0=g8[:, :, 0], in1=g8[:, :, 1], op=ALU.add),
             reads=["g8"], writes=["gs"])
        P.op("dve", lambda e: e.max(out=gs8[:], in_=gs[:]), reads=["gs"], writes=["gs8"])
        P.op("dve", lambda e: e.tensor_scalar(out=pen[:], in0=gs[:], scalar1=gs8[:, 3:4], scalar2=-1.0e9,
                                              op0=ALU.is_lt, op1=ALU.mult), reads=["gs", "gs8"], writes=["pen"])
        P.op("dve", lambda e: e.tensor_tensor(out=choice[:].rearrange("p (g j) -> p g j", j=32),
                                              in0=biased[:].rearrange("p (g j) -> p g j", j=32),
                                              in1=pen[:].unsqueeze(2).broadcast_to([128, 8, 32]), op=ALU.add),
             reads=["biased", "pen"], writes=["choice"])
        P.op("dve", lambda e: e.max(out=t8[:], in_=choice[:]), reads=["choice"], writes=["t8"])
        P.op("dve", lambda e: e.tensor_scalar(out=Mf[:], in0=choice[:], scalar1=t8[:, 7:8], scalar2=None,
                                              op0=ALU.is_ge), reads=["choice", "t8"], writes=["Mf"])
        P.op("pool", lambda e, i=i: e.tensor_copy(out=Mall[:, i, :], in_=Mf[:]), reads=["Mf"], writes=[f"Mall{i}"])
        P.op("dve", lambda e: e.tensor_tensor(out=ws_t[:], in0=sc_t2[i % 2][:], in1=Mf[:], op=ALU.mult),
             reads=[f"sc_t{i % 2}", "Mf"], writes=["ws_t"])
        P.op("dve", lambda e, i=i: e.tensor_reduce(out=sm[:, 4 * i + 3:4 * i + 4], in_=ws_t[:], axis=AX.X,
                                                   op=ALU.add), reads=["ws_t"], writes=[f"sm{i}d"])
        P.op("dve", lambda e, i=i: e.reciprocal(out=sm[:, 4 * i + 3:4 * i + 4], in_=sm[:, 4 * i + 3:4 * i + 4]),
             reads=[f"sm{i}d"], writes=[f"sm{i}d"])
        P.op("dve", lambda e, i=i: e.tensor_scalar(out=Wd[:], in0=ws_t[:], scalar1=sm[:, 4 * i + 3:4 * i + 4],
                                                   scalar2=2.5, op0=ALU.mult, op1=ALU.mult),
             reads=["ws_t", f"sm{i}d"], writes=["Wd"])
        d = dbg.get("Wd")
        if d is not None:
            P.op("sp", lambda e, d=d, i=i: e.dma_start(out=d[i * 128:(i + 1) * 128, :], in_=Wd[:]), reads=["Wd"],
                 dma="dbg8")
        P.op("pe", lambda e, i=i: e.matmul(psf(4)[:, 0:NEXP], lhsT=Ltri[:], rhs=Mall[:, i, :], start=True,
                                           stop=True), reads=["Ltri", f"Mall{i}"], writes=["ps4"])
        P.op("dve", lambda e: e.tensor_tensor(out=keyt[:], in0=psf(4)[:, 0:NEXP], in1=cnt_bc[:], op=ALU.add),
             reads=["ps4", "cnt_bc"], writes=["keyt"])
        P.op("pe", lambda e, i=i: e.matmul(psf(5)[:, 0:NEXP], lhsT=ones_bf[:], rhs=Mall[:, i, :], start=True,
                                           stop=True), reads=["ones_bf", f"Mall{i}"], writes=["ps5"])
        P.op("dve", lambda e: e.tensor_tensor(out=cnt_bc[:], in0=psf(5)[:, 0:NEXP], in1=cnt_bc[:], op=ALU.add),
             reads=["ps5", "cnt_bc"], writes=["cnt_bc"])
        P.op("dve", lambda e: e.tensor_scalar(out=jk[:], in0=keyt[:], scalar1=float(CAP), scalar2=1.0e6,
                                              op0=ALU.is_ge, op1=ALU.mult), reads=["keyt"], writes=["jk"])
        P.op("pool", lambda e: e.tensor_tensor(out=keyt[:], in0=keyt[:], in1=base_e[:], op=ALU.add),
             reads=["keyt", "base_e"], writes=["keyt"])
        P.op("pool", lambda e: e.tensor_tensor(out=keyt[:], in0=keyt[:], in1=jk[:], op=ALU.add),
             reads=["keyt", "jk"], writes=["keyt"])
        P.op("dve", lambda e: e.tensor_tensor(out=keyt[:], in0=keyt[:], in1=Mf[:], op=ALU.mult),
             reads=["keyt", "Mf"], writes=["keyt"])
        P.op("dve", lambda e: e.max(out=key8[:], in_=keyt[:]), reads=["keyt"], writes=["key8"])
        P.op("dve", lambda e, i=i: e.tensor_scalar(out=dest8[:, i, :], in0=key8[:], scalar1=-1.0, scalar2=None,
                                                   op0=ALU.add), reads=["key8"], writes=[f"dest8_{i}"])
        for k in range(8):
            P.op("dve", lambda e, i=i, k=k: e.scalar_tensor_tensor(
                out=jk[:], in0=keyt[:], scalar=key8[:, k:k + 1], in1=Wd[:], op0=ALU.is_equal, op1=ALU.mult,
                accum_out=W8[:, i, k:k + 1]), reads=["keyt", "key8", "Wd"], writes=["jk", f"W8_{i}"])
        P.op("dve", lambda e: e.tensor_scalar(out=gs8[:], in0=key8[:], scalar1=1.0e6, scalar2=None, op0=ALU.is_lt),
             reads=["key8"], writes=["gs8"])
        P.op("dve", lambda e, i=i: e.tensor_tensor(out=W8[:, i, :], in0=W8[:, i, :], in1=gs8[:], op=ALU.mult),
             reads=["gs8", f"W8_{i}"], writes=[f"W8_{i}"])
        P.op("sp", lambda e, i=i, b=b: e.dma_start(out=h2_d[i * 128:(i + 1) * 128, :], in_=h2b[b][:]),
             reads=[f"h2b{b}"], dma=f"d_h2w{b}")
        for k in range(8):
            P.op("pool", lambda e, i=i, k=k: e.indirect_dma_start(
                out=tokidx_d[:, :], out_offset=bass.IndirectOffsetOnAxis(ap=dest8[:, i, k:k + 1], axis=0),
                in_=tokid[:, i, k, :], in_offset=None, bounds_check=P.reg(e, NEXP * CAP - 1), oob_is_err=False),
                reads=[f"dest8_{i}", "tokid"], dma="d_scat", extra=[tok_init])
    f_stageA(0)
    for i_ in range(NT):
        if i_ + 1 < NT:
            f_stageA(i_ + 1)
        f_stageB(i_)
    if "Wd" in dbg:
        out_keys.append("dbg8")
    d = dbg_out("h2T", [128, KC * S], BF16)
    if d is not None:
        P.op("sp", lambda e, d=d: e.dma_start(out=d, in_=hT[:].rearrange("p a b -> p (a b)")),
             reads=[f"hT{i}" for i in range(NT)], dma="dbg9")
        out_keys.append("dbg9")
    d = dbg_out("dest8", [128, NT * 8], I32)
    if d is not None:
        P.op("sp", lambda e, d=d: e.dma_start(out=d, in_=dest8[:].rearrange("p a b -> p (a b)")),
             reads=[f"dest8_{i}" for i in range(NT)], dma="dbg10")
        out_keys.append("dbg10")
    d = dbg_out("W8", [128, NT * 8], F32)
    if d is not None:
        P.op("sp", lambda e, d=d: e.dma_start(out=d, in_=W8[:].rearrange("p a b -> p (a b)")),
             reads=[f"W8_{i}" for i in range(NT)], dma="dbg11")
        out_keys.append("dbg11")
    P.barrier()
    P.release(mF)
    if stop_after == "F":
        return finish()


    NE_RUN = int(os.environ.get("NE_RUN", str(NEXP)))
    mE = P.mark()
    top_off = P.sb_off
    P.sb_off = cat_off
    wgu = [P.sb(f"wgu{i}", [128, 2, KC, 256], BF16) for i in range(3)]
    xgb = [P.sb(f"xg{i}", [128, 2, D], BF16) for i in range(2)]
    assert P.sb_off <= cat_end
    P.sb_off = top_off
    xgb.append(P.sb("xg2", [128, 2, D], BF16))
    wdn = [P.sb(f"wdn{i}", [128, 2, D], BF16) for i in range(3)]
    xTb = [P.sb(f"xTb{i}", [128, KC, CAP], BF16) for i in range(2)]
    sgate = [P.sb(f"sgate{i}", [128, 512], F32) for i in range(2)]
    hidT = [P.sb(f"hidT{i}", [128, 2, CAP], BF16) for i in range(2)]
    obuf = [P.sb(f"obuf{i}", [128, 2, D], BF16) for i in range(2)]

    def load_expert_w(ex):
        sl = ex % 3
        P.op("pool", lambda e, ex=ex, sl=sl: e.dma_start(
            out=wgu[sl][:, 0], in_=w_gate_d[ex].rearrange("(p k) f -> p k f", p=128)),
            writes=[f"wgu{sl}"], dma=f"d_wg{sl}")
        P.op("pool", lambda e, ex=ex, sl=sl: e.dma_start(
            out=wgu[sl][:, 1], in_=w_up_d[ex].rearrange("(p k) f -> p k f", p=128)),
            writes=[f"wgu{sl}b"], dma=f"d_wu{sl}")
        P.op("pool", lambda e, ex=ex, sl=sl: e.dma_start(
            out=wdn[sl][:], in_=w_down_d[ex].rearrange("(k p) f -> p k f", p=128)),
            writes=[f"wdn{sl}"], dma=f"d_wd{sl}")

    NIDX = 6
    idxb = [P.sb(f"idxb{i}", [128, 2, 2], I32) for i in range(NIDX)]
    for sl_ in range(3):
        P.op("pool", lambda e, sl_=sl_: e.memset(xgb[sl_][:].rearrange("p b d -> p (b d)"), 0.0),
             writes=[f"xg{sl_}_0", f"xg{sl_}_1"])

    def load_expert_idx(ex):
        s3 = ex % NIDX
        for blk in range(2):
            P.op("sp", lambda e, ex=ex, s3=s3, blk=blk: e.dma_start(
                out=idxb[s3][:, blk, :], in_=tokidx_d[ex * CAP + blk * 128: ex * CAP + (blk + 1) * 128, :]),
                writes=[f"idxb{s3}_{blk}"], dma=f"d_idx{s3}_{blk}")

    def load_expert_x(ex):
        sl = ex % 3
        s3 = ex % NIDX
        for blk in range(2):
            P.op("pool", lambda e, sl=sl, s3=s3, blk=blk: e.indirect_dma_start(
                out=xgb[sl][:, blk, :], out_offset=None, in_=h2_d[:, :],
                in_offset=bass.IndirectOffsetOnAxis(ap=idxb[s3][:, blk, 0:1], axis=0),
                bounds_check=P.reg(e, S - 1), oob_is_err=False),
                reads=[f"idxb{s3}_{blk}"], writes=[f"xg{sl}_{blk}"], dma=f"d_xg{sl}_{blk}")

    def ex_T(ex):
        sl = ex % 3
        for blk in range(2):
            fns = [lambda e, c=c, blk=blk: e.transpose(
                out=psh(blk)[:, c * 128:(c + 1) * 128],
                in_=xgb[sl][:, blk, :].rearrange("p (q k) -> p k q", k=KC)[:, c, :],
                identity=ident_bf[:]) for c in range(KC)]
            P.group("pe", fns, reads=[f"xg{sl}_{blk}", "ident_bf"], writes=[f"ps{blk}"])

    def ex_T_evac(ex):
        sl = ex % 2
        P.op("act", lambda e: e.copy(out=xTb[sl][:, :, 0:128], in_=psh(0)[:, :].rearrange("p (a b) -> p a b", b=128)),
             reads=["ps0"], writes=[f"xT{sl}_0"])
        P.op("dve", lambda e: e.tensor_copy(out=xTb[sl][:, :, 128:256],
                                            in_=psh(1)[:, :].rearrange("p (a b) -> p a b", b=128)),
             reads=["ps1"], writes=[f"xT{sl}_1"])

    def ex_GU(ex):
        sl = ex % 2
        sl3 = ex % 3
        for fc in range(4):
            bank = 2 + fc // 2
            co = (fc % 2) * 256
            fns = [lambda e, k=k, fc=fc, bank=bank, co=co: e.matmul(
                psf(bank)[:, co:co + 256], lhsT=wgu[sl3][:, fc // 2, k, (fc % 2) * 128:(fc % 2 + 1) * 128],
                rhs=xTb[sl][:, k, :],
                start=(k == 0), stop=(k == KC - 1)) for k in range(KC)]
            P.group("pe", fns, reads=[f"wgu{sl3}", f"wgu{sl3}b", f"xT{sl}_0", f"xT{sl}_1"],
                    writes=[f"ps{bank}"])

    def ex_act(ex):
        sl = ex % 2
        P.op("act", lambda e: e.activation(out=sgate[sl][:], in_=psf(2)[:, :], func=AF.Silu),
             reads=["ps2"], writes=[f"sgate{sl}"])
        P.op("dve", lambda e: e.tensor_tensor(out=hidT[sl][:].rearrange("p a b -> p (a b)"),
                                              in0=psf(3)[:, :], in1=sgate[sl][:], op=ALU.mult),
             reads=["ps3", f"sgate{sl}"], writes=[f"hidT{sl}"])

    def ex_D(ex):
        sl = ex % 2
        sl3 = ex % 3
        for blk in range(2):
            for hh in range(2):
                bank = 4 + blk * 2 + hh
                fns = [lambda e, fc=fc, blk=blk, hh=hh, bank=bank: e.matmul(
                    psf(bank)[:, :], lhsT=hidT[sl][:, fc, blk * 128:(blk + 1) * 128],
                    rhs=wdn[sl3][:, fc, hh * 512:(hh + 1) * 512], start=(fc == 0), stop=(fc == 1))
                    for fc in range(2)]
                P.group("pe", fns, reads=[f"hidT{sl}", f"wdn{sl3}"], writes=[f"ps{bank}"])

    def ex_out(ex):
        sl = ex % 2
        for blk in range(2):
            for hh in range(2):
                bank = 4 + blk * 2 + hh
                if (blk * 2 + hh) % 2 == 0:
                    P.op("act", lambda e, blk=blk, hh=hh, bank=bank: e.copy(
                        out=obuf[sl][:, blk, hh * 512:(hh + 1) * 512], in_=psf(bank)[:, :]),
                        reads=[f"ps{bank}"], writes=[f"obuf{sl}"])
                else:
                    P.op("dve", lambda e, blk=blk, hh=hh, bank=bank: e.tensor_copy(
                        out=obuf[sl][:, blk, hh * 512:(hh + 1) * 512], in_=psf(bank)[:, :]),
                        reads=[f"ps{bank}"], writes=[f"obuf{sl}"])
        s3 = ex % NIDX
        for blk in range(2):
            P.op("pool", lambda e, blk=blk: e.indirect_dma_start(
                out=o8_d[:, :], out_offset=bass.IndirectOffsetOnAxis(ap=idxb[s3][:, blk, 1:2], axis=0),
                in_=obuf[sl][:, blk, :], in_offset=None, bounds_check=P.reg(e, S * 8 - 1), oob_is_err=False),
                reads=[f"obuf{sl}", f"idxb{s3}_{blk}"], dma=f"d_ob{sl}_{blk}", extra=[o8_init_tok])

    for e0 in range(min(3, NE_RUN)):
        load_expert_idx(e0)
    for e0 in range(min(2, NE_RUN)):
        load_expert_x(e0)
        load_expert_w(e0)
    if NE_RUN > 0:
        ex_T(0)
        ex_T_evac(0)
    for ex in range(NE_RUN):
        if ex + 3 < NE_RUN:
            load_expert_idx(ex + 3)
        if ex + 2 < NE_RUN:
            load_expert_x(ex + 2)
            load_expert_w(ex + 2)
        ex_GU(ex)
        if ex + 1 < NE_RUN:
            ex_T(ex + 1)
        ex_act(ex)
        if ex + 1 < NE_RUN:
            ex_T_evac(ex + 1)
        ex_D(ex)
        ex_out(ex)
    P.barrier()
    P.release(mE)
    if stop_after == "E":
        return finish()

    top_off = P.sb_off
    P.sb_off = cat_off
    gk = [P.sb(f"gk{r}", [128, 8, D], BF16) for r in range(2)]
    assert P.sb_off <= cat_end
    P.sb_off = top_off
    wsgu = P.sb("wsgu", [128, KC, 512], BF16)
    wsd = P.sb("wsd", [128, 2, D], BF16)
    diag = [P.sb(f"diag{r}", [128, 8, 128], BF16) for r in range(2)]
    sgs = [P.sb(f"sgs{r}", [128, EFF], F32) for r in range(2)]
    hs_tok = [P.sb(f"hs_tok{r}", [128, EFF], BF16) for r in range(2)]
    hsT = [P.sb(f"hsT{r}", [128, 2, 128], BF16) for r in range(2)]
    ytmp = [P.sb(f"ytmp{r}", [128, 512], F32) for r in range(2)]
    P.op("pool", lambda e: e.dma_start(out=wsgu[:, :, 0:256], in_=ws_gate_d.rearrange("(k p) f -> p k f", p=128)),
         writes=["wsgu_a"], dma="c9")
    P.op("pool", lambda e: e.dma_start(out=wsgu[:, :, 256:512], in_=ws_up_d.rearrange("(k p) f -> p k f", p=128)),
         writes=["wsgu_b"], dma="c10")
    P.op("pool", lambda e: e.dma_start(out=wsd[:], in_=ws_down_d.rearrange("(k p) f -> p k f", p=128)),
         writes=["wsd"], dma="c11")
    def c_load(i):
        r = i % 2
        P.op("sp", lambda e: e.dma_start(
            out=gk[r][:], in_=o8_d[i * 1024:(i + 1) * 1024, :].rearrange("(p k) d -> p k d", k=8)),
            writes=[f"gk{r}"], dma=f"d_gk{r}")

    def c_diag(i):
        r = i % 2
        for k in range(8):
            if k % 2 == 0:
                P.op("dve", lambda e, k=k: e.tensor_scalar(out=diag[r][:, k, :], in0=ident_bf[:],
                                                           scalar1=W8[:, i, k:k + 1], scalar2=None, op0=ALU.mult),
                     reads=["ident_bf"], writes=[f"diag{r}_{k}"])
            else:
                P.op("act", lambda e, k=k: e.activation(out=diag[r][:, k, :], in_=ident_bf[:], func=AF.Copy,
                                                        scale=W8[:, i, k:k + 1]),
                     reads=["ident_bf"], writes=[f"diag{r}_{k}"])

    def c_shared(i):
        r = i % 2
        fns = [lambda e, k=k: e.matmul(psf(0)[:, :], lhsT=hT[:, k, i * 128:(i + 1) * 128], rhs=wsgu[:, k, :],
                                       start=(k == 0), stop=(k == KC - 1)) for k in range(KC)]
        P.group("pe", fns, reads=["wsgu_a", "wsgu_b"], writes=["ps0"])
        P.op("act", lambda e: e.activation(out=sgs[r][:], in_=psf(0)[:, 0:EFF], func=AF.Silu), reads=["ps0"],
             writes=[f"sgs{r}"])
        P.op("dve", lambda e: e.tensor_tensor(out=hs_tok[r][:], in0=psf(0)[:, EFF:2 * EFF], in1=sgs[r][:],
                                              op=ALU.mult), reads=["ps0", f"sgs{r}"], writes=[f"hs_tok{r}"])
        fns = [lambda e, c=c: e.transpose(out=psh(1)[:, c * 128:(c + 1) * 128],
                                          in_=hs_tok[r][:, c * 128:(c + 1) * 128], identity=ident_bf[:])
               for c in range(2)]
        P.group("pe", fns, reads=[f"hs_tok{r}", "ident_bf"], writes=["ps1"])
        P.op("act", lambda e: e.copy(out=hsT[r][:], in_=psh(1)[:, 0:256].rearrange("p (a b) -> p a b", b=128)),
             reads=["ps1"], writes=[f"hsT{r}"])

    def c_final(i):
        r = i % 2
        for hh in range(2):
            bank = 2 + ((2 * i + hh) % 4)
            fns = [lambda e, fc=fc, hh=hh, bank=bank: e.matmul(
                psf(bank)[:, :], lhsT=hsT[r][:, fc, :], rhs=wsd[:, fc, hh * 512:(hh + 1) * 512], start=(fc == 0),
                stop=False) for fc in range(2)]
            fns += [lambda e, k=k, hh=hh, bank=bank: e.matmul(
                psf(bank)[:, :], lhsT=diag[r][:, k, :], rhs=gk[r][:, k, hh * 512:(hh + 1) * 512], start=False,
                stop=(k == 7)) for k in range(8)]
            P.group("pe", fns, reads=[f"hsT{r}", "wsd"] + [f"diag{r}_{k}" for k in range(8)] + [f"gk{r}"],
                    writes=[f"ps{bank}"])
            yb = (2 * i + hh) % 2
            P.op("dve", lambda e, hh=hh, bank=bank, yb=yb: e.tensor_tensor(
                out=ytmp[yb][:], in0=psf(bank)[:, :], in1=bc[:, 1, hh * 512:(hh + 1) * 512], op=ALU.mult),
                reads=[f"ps{bank}", "bc"], writes=[f"ytmp{yb}"])
            P.op("pool", lambda e, hh=hh, yb=yb: e.tensor_tensor(
                out=xres[:, i, hh * 512:(hh + 1) * 512], in0=ytmp[yb][:], in1=xres[:, i, hh * 512:(hh + 1) * 512],
                op=ALU.add), reads=[f"ytmp{yb}"], writes=[f"xres{i}"])
        P.op("sp", lambda e: e.dma_start(out=y_d[i * 128:(i + 1) * 128, :], in_=xres[:, i, :]),
             reads=[f"xres{i}"], dma="d_y")

    c_load(0)
    c_shared(0)
    for i in range(NT):
        if i + 1 < NT:
            c_load(i + 1)
        c_diag(i)
        if i + 1 < NT:
            c_shared(i + 1)
        c_final(i)
    out_keys.append("d_y")

    return finish()


def build_nc_2pass(debug=(), stop_after=None):
    _LAYOUT.clear()
    build_nc(debug=(), stop_after="R")
    _LAYOUT["tbl"] = _LAYOUT["tbl_new"]
    _LAYOUT["tbl_small"] = _LAYOUT["tbl_small_new"]
    return build_nc(debug=debug, stop_after=stop_after)


def make_in_maps(inputs):
    f = lambda a: np.ascontiguousarray(np.asarray(a, dtype=np.float32))
    x = f(inputs["x"])
    c = f(inputs["c"])
    w_in = f(inputs["w_in"])[0]
    perm = np.concatenate([np.arange(64, 128), np.arange(0, 64)])
    cols = []
    rq = w_in[:, 0:512].reshape(D, 4, 128)
    rk = w_in[:, 512:1024].reshape(D, 4, 128)
    cols.append(rq.reshape(D, 512))
    cols.append(rq[:, :, perm].reshape(D, 512))
    cols.append(rk.reshape(D, 512))
    cols.append(rk[:, :, perm].reshape(D, 512))
    cols.append(w_in[:, 1024:3584])
    w_in_x = np.ascontiguousarray(np.concatenate(cols, axis=1))
    shared = {
        "w_ada": f(inputs["w_ada"])[0],
        "b_ada": f(inputs["b_ada"])[0].reshape(1, 6 * D),
        "g_mix_c": np.ascontiguousarray(f(inputs["g_mix"])[0].reshape(KC, 128).T),
        "g_ffn_c": np.ascontiguousarray(f(inputs["g_ffn"])[0].reshape(KC, 128).T),
        "g_ffn_r": f(inputs["g_ffn"])[0].reshape(1, D),
        "w_in_x": w_in_x,
        "q_gain": f(inputs["q_gain"])[0].reshape(1, 64),
        "k_gain": f(inputs["k_gain"])[0].reshape(1, 64),
        "w_out": f(inputs["w_out"])[0],
        "w_router": f(inputs["w_router"])[0],
        "router_bias": f(inputs["router_bias"])[0].reshape(1, NEXP),
        "w_gate": f(inputs["w_gate"])[0],
        "w_up": f(inputs["w_up"])[0],
        "w_down": f(inputs["w_down"])[0],
        "ws_gate": f(inputs["ws_gate"])[0],
        "ws_up": f(inputs["ws_up"])[0],
        "ws_down": f(inputs["ws_down"])[0],
    }
    maps = []
    for b in range(x.shape[0]):
        m = dict(shared)
        m["x"] = x[b]
        m["cT"] = np.ascontiguousarray(c[b].reshape(KC, 128).T)
        maps.append(m)
    return maps


_NC_CACHE = {}
_LAYOUT = {}


def kernel(**inputs):
    maps = make_in_maps(inputs)
    if "nc" not in _NC_CACHE:
        _NC_CACHE["nc"] = build_nc_2pass()[0]
    nc = _NC_CACHE["nc"]
    res = run_bass_kernel_spmd(nc, maps, core_ids=list(range(8)))
    return np.stack([np.asarray(r["y"], dtype=np.float32) for r in res.results], axis=0)
```

```python
import numpy as np
import concourse.bass as bass
import concourse.mybir as mybir
from concourse.bass_utils import run_bass_kernel_spmd

F32 = mybir.dt.float32
BF16 = mybir.dt.bfloat16
I32 = mybir.dt.int32
U32 = mybir.dt.uint32
AF = mybir.ActivationFunctionType
ALU = mybir.AluOpType
AX = mybir.AxisListType

S = 2048
D = 1024
NT = 16
KC = 8
EPS = 1e-6
NEXP = 256
CAP = 256
EFF = 256


class Stream:
    def __init__(self, name):
        self.name = name
        self.items = []
        self.waited = {}


class Prog:
    ENG = ("pe", "act", "dve", "pool", "sp")
    import os as _os
    NO_SELF_WAIT = tuple(_os.environ.get("NO_SELF_WAIT", "pe").split(","))

    def __init__(self, nc):
        self.nc = nc
        self.streams = {n: Stream(n) for n in self.ENG}
        self.sems = {}
        self.semval = {}
        self.last_w = {}
        self.readers = {}
        self.sb_off = 16512
        self.n_ops = 0

    def sb(self, name, shape, dtype):
        esz = 4 if dtype in (F32, I32, U32) else 2
        n = 1
        for s in shape[1:]:
            n *= s
        nbytes = (n * esz + 63) // 64 * 64
        off = self.sb_off
        assert off + nbytes <= 229344, f"SBUF overflow at {name}: {off}+{nbytes}"
        self.sb_off += nbytes
        return self.nc.alloc_sbuf_tensor_at(name, list(shape), dtype, offset=off)

    def mark(self):
        return self.sb_off

    def release(self, m):
        self.sb_off = m

    def _sem(self, key):
        if key not in self.sems:
            self.sems[key] = self.nc.alloc_semaphore("s_" + key)
            self.semval[key] = 0
        return self.sems[key]

    def _wait(self, st, tok):
        if tok is None:
            return
        key, val = tok
        if key == "e_" + st.name and st.name in self.NO_SELF_WAIT:
            return
        if st.waited.get(key, 0) >= val:
            return
        st.waited[key] = val
        st.items.append(("wait", key, val))

    def _wait_all(self, st, deps):
        mx = {}
        for d in deps:
            if d is None:
                continue
            if d[1] > mx.get(d[0], 0):
                mx[d[0]] = d[1]
        for k, v in mx.items():
            self._wait(st, (k, v))

    def wait(self, eng, tok):
        self._wait(self.streams[eng], tok)

    def op(self, eng, fn, reads=(), writes=(), dma=None, sig=True, extra=()):
        st = self.streams[eng]
        deps = list(extra)
        for r in reads:
            deps.append(self.last_w.get(r))
        for w in writes:
            deps.append(self.last_w.get(w))
            deps.extend(self.readers.get(w, ()))
        self._wait_all(st, deps)
        tok = None
        key = None
        inc = 1
        if dma is not None:
            key, inc = dma, 16
        elif sig:
            key = "e_" + eng
        if key is not None:
            self._sem(key)
            self.semval[key] += inc
            tok = (key, self.semval[key])
        st.items.append(("op", fn, key, inc))
        self.n_ops += 1
        if tok is not None:
            for r in reads:
                self.readers.setdefault(r, []).append(tok)
            for w in writes:
                self.last_w[w] = tok
                self.readers[w] = []
        return tok

    def group(self, eng, fns, reads=(), writes=(), extra=()):
        st = self.streams[eng]
        deps = list(extra)
        for r in reads:
            deps.append(self.last_w.get(r))
        for w in writes:
            deps.append(self.last_w.get(w))
            deps.extend(self.readers.get(w, ()))
        self._wait_all(st, deps)
        for fn in fns[:-1]:
            st.items.append(("op", fn, None, 1))
        return self.op(eng, fns[-1], reads=reads, writes=writes)

    def replay(self, eng, e):
        for it in self.streams[eng].items:
            if it[0] == "wait":
                e.wait_ge(self.sems[it[1]], it[2])
            else:
                ins = it[1](e)
                if it[2] is not None:
                    ins.then_inc(self.sems[it[2]], it[3])

    def barrier(self):
        st = self.streams["act"]
        for k, v in self.semval.items():
            if v > 0:
                self._wait(st, (k, v))
        tok = self.op("act", lambda e: e.copy(out=self.bar_scratch[:, 0:1], in_=self.bar_scratch[:, 1:2]))
        for n, s2 in self.streams.items():
            self._wait(s2, tok)
        self.last_w = {}
        self.readers = {}
        return tok

    def reg(self, e, val):
        if not hasattr(self, "_regs"):
            self._regs = {}
        if val not in self._regs:
            self._regs[val] = e.to_reg(val)
        return self._regs[val]

    def final_waits(self, eng, keys):
        st = self.streams[eng]
        for k in keys:
            if k in self.semval and self.semval[k] > 0:
                self._wait(st, (k, self.semval[k]))


def build_nc(debug=(), stop_after=None):
    import os
    import math
    BIS = int(os.environ.get('BIS', '99'))
    nc = bass.Bass("TRN2", target_bir_lowering=False)
    P = Prog(nc)

    def din(name, shape, dt=F32):
        return nc.dram_tensor(name, list(shape), dt, kind="ExternalInput").ap()

    x_d = din("x", [S, D])
    cT_d = din("cT", [128, KC])
    w_ada_d = din("w_ada", [D, 6 * D])
    b_ada_d = din("b_ada", [1, 6 * D])
    gmix_d = din("g_mix_c", [128, KC])
    gffn_d = din("g_ffn_c", [128, KC])
    gffn_r_d = din("g_ffn_r", [1, D])
    w_in_d = din("w_in_x", [D, 4608])
    qg_d = din("q_gain", [1, 64])
    kg_d = din("k_gain", [1, 64])
    w_out_d = din("w_out", [D, D])
    w_router_d = din("w_router", [D, NEXP])
    rbias_d = din("router_bias", [1, NEXP])
    if stop_after is None:
        w_gate_d = din("w_gate", [NEXP, D, EFF])
        w_up_d = din("w_up", [NEXP, D, EFF])
        w_down_d = din("w_down", [NEXP, EFF, D])
    ws_gate_d = din("ws_gate", [D, EFF])
    ws_up_d = din("ws_up", [D, EFF])
    ws_down_d = din("ws_down", [EFF, D])
    y_d = nc.dram_tensor("y", [S, D], F32, kind="ExternalOutput").ap()

    dbg = {}

    def dbg_out(name, shape, dt=F32):
        if name in debug:
            dbg[name] = nc.dram_tensor("dbg_" + name, list(shape), dt, kind="ExternalOutput").ap()
            return dbg[name]
        return None

    h2_d = nc.dram_tensor("h2_scr", [S, D], BF16).ap()
    tokidx_d = nc.dram_tensor("tokidx_scr", [NEXP * CAP, 2], I32).ap()
    o8_d = nc.dram_tensor("o8_scr", [S * 8, D], BF16).ap()

    ps = [nc.alloc_psum_tensor(f"psb{i}", [128, 512], F32) for i in range(8)]

    def psf(b):
        return ps[b][:]

    def psh(b):
        return ps[b][:].bitcast(BF16)

    out_keys = []

    def finish():
        P.final_waits("sp", out_keys)
        with nc.Block() as block:
            @block.tensor
            def _(e):
                P.replay("pe", e)

            @block.scalar
            def _(e):
                P.replay("act", e)

            @block.vector
            def _(e):
                P.replay("dve", e)

            @block.gpsimd
            def _(e):
                P.replay("pool", e)

            @block.sync
            def _(e):
                P.replay("sp", e)
        return nc, P


    ident_bf = P.sb("ident_bf", [128, 128], BF16)
    ident_f = P.sb("ident_f", [128, 128], F32)
    ones_f = P.sb("ones_f", [128, 128], F32)
    ones_bf = P.sb("ones_bf", [128, 128], BF16)
    modT = P.sb("modT", [128, 48], F32)
    A1 = P.sb("A1", [128, KC], F32)
    A2 = P.sb("A2", [128, KC], F32)
    gmix = P.sb("gmix", [128, KC], F32)
    gffn = P.sb("gffn", [128, KC], F32)
    bc = P.sb("bc", [128, 4, D], F32)
    stat = P.sb("stat", [128, 64], F32)
    P.bar_scratch = P.sb("bar_scratch", [128, 2], F32)
    epsb = P.sb("epsb", [128, 1], F32)

    P.op("pool", lambda e: e.memset(ones_f[:], 1.0), writes=["ones_f"])
    P.op("pool", lambda e: e.memset(epsb[:], EPS), writes=["epsb"])
    P.op("pool", lambda e: e.memset(P.bar_scratch[:], 0.0), writes=["bar_scratch"])
    P.op("pool", lambda e: e.memset(ones_bf[:], 1.0), writes=["ones_bf"])
    P.op("pool", lambda e: e.memset(ident_f[:], 0.0), writes=["ident_f"])
    P.op("pool", lambda e: e.affine_select(out=ident_f[:], in_=ones_f[:], pattern=[[-1, 128]],
                                           compare_op=ALU.is_equal, fill=0.0, base=0,
                                           channel_multiplier=1),
         reads=["ones_f"], writes=["ident_f"])
    P.op("dve", lambda e: e.tensor_copy(out=ident_bf[:], in_=ident_f[:]), reads=["ident_f"],
         writes=["ident_bf"])

    T_ = {}
    LNG = [math.log1p(-2.0 ** (-5.0 - h)) for h in range(4)]
    DKS = 128.0 ** -0.5

    def build_tables():
        cosT = P.sb("cosT", [128, S], F32)
        sinT = P.sb("sinT", [128, S], F32)
        mT = P.mark()
        ti = P.sb("ti", [128, S], I32)
        tf = P.sb("tf", [128, S], F32)
        ang = P.sb("ang", [128, S], F32)
        tmpA = P.sb("tmpA", [128, S], F32)
        ji = P.sb("ji", [128, 1], I32)
        jf = P.sb("jf", [128, 1], F32)
        inv = P.sb("inv", [128, 1], F32)
        sgn = P.sb("sgn", [128, 1], F32)
        pi_ = P.sb("pi_", [128, 1], I32)
        pf = P.sb("pf", [128, 1], F32)
        ci = P.sb("ci", [128, 128], I32)
        cf = P.sb("cf", [128, 128], F32)
        biasc = P.sb("biasc", [128, 8], F32)
        PI = math.pi
        for half in range(2):
            P.op("pool", lambda e, half=half: e.iota(out=ji[half * 64:(half + 1) * 64, :], pattern=[[0, 1]], base=0,
                                                    channel_multiplier=1), writes=["ji"])
        P.op("pool", lambda e: e.memset(sgn[0:64, :], -1.0), writes=["sgn"])
        P.op("pool", lambda e: e.memset(sgn[64:128, :], 1.0), writes=["sgn"])
        P.op("dve", lambda e: e.tensor_copy(out=jf[:], in_=ji[:]), reads=["ji"], writes=["jf"])
        P.op("act", lambda e: e.activation(out=inv[:], in_=jf[:], func=AF.Exp, scale=-math.log(10000.0) / 64.0),
             reads=["jf"], writes=["inv"])
        P.op("pool", lambda e: e.iota(out=ti[:], pattern=[[1, S]], base=0, channel_multiplier=0), writes=["ti"])
        P.op("dve", lambda e: e.tensor_copy(out=tf[:], in_=ti[:]), reads=["ti"], writes=["tf"])
        P.op("dve", lambda e: e.tensor_scalar(out=ang[:], in0=tf[:], scalar1=inv[:, 0:1], scalar2=None, op0=ALU.mult),
             reads=["tf", "inv"], writes=["ang"])

        def range_reduce_sin(dst, shift, scale_ap, dname):
            src = "ang"
            if shift != 0.0:
                P.op("pool", lambda e: e.tensor_scalar(out=tmpA[:], in0=ang[:], scalar1=shift, scalar2=None,
                                                       op0=ALU.add), reads=["ang"], writes=["tmpA"])
                a = tmpA
                src = "tmpA"
            else:
                a = ang
            P.op("dve", lambda e: e.tensor_scalar(out=tf[:], in0=a[:], scalar1=1.0 / (2 * PI), scalar2=None,
                                                  op0=ALU.mult), reads=[src], writes=["tf"])
            P.op("dve", lambda e: e.tensor_copy(out=ti[:], in_=tf[:]), reads=["tf"], writes=["ti"])
            P.op("dve", lambda e: e.tensor_copy(out=tf[:], in_=ti[:]), reads=["ti"], writes=["tf"])
            C1 = 6.28125
            C2 = 2 * PI - C1
            P.op("dve", lambda e: e.scalar_tensor_tensor(out=tmpA[:], in0=tf[:], scalar=-C1, in1=a[:], op0=ALU.mult,
                                                         op1=ALU.add), reads=["tf", src], writes=["tmpA"])
            P.op("dve", lambda e: e.scalar_tensor_tensor(out=tmpA[:], in0=tf[:], scalar=-C2, in1=tmpA[:], op0=ALU.mult,
                                                         op1=ALU.add), reads=["tf", "tmpA"], writes=["tmpA"])
            P.op("dve", lambda e: e.tensor_scalar(out=tf[:], in0=tmpA[:], scalar1=PI, scalar2=-2 * PI, op0=ALU.is_gt,
                                                  op1=ALU.mult), reads=["tmpA"], writes=["tf"])
            P.op("pool", lambda e: e.tensor_tensor(out=tmpA[:], in0=tmpA[:], in1=tf[:], op=ALU.add),
                 reads=["tmpA", "tf"], writes=["tmpA"])
            P.op("dve", lambda e: e.tensor_scalar(out=tf[:], in0=tmpA[:], scalar1=-PI, scalar2=2 * PI, op0=ALU.is_lt,
                                                  op1=ALU.mult), reads=["tmpA"], writes=["tf"])
            P.op("pool", lambda e: e.tensor_tensor(out=tmpA[:], in0=tmpA[:], in1=tf[:], op=ALU.add),
                 reads=["tmpA", "tf"], writes=["tmpA"])
            P.op("dve", lambda e: e.tensor_scalar(out=tmpA[:], in0=tmpA[:], scalar1=PI, scalar2=-PI, op0=ALU.min,
                                                  op1=ALU.max), reads=["tmpA"], writes=["tmpA"])
            if scale_ap is None:
                P.op("act", lambda e: e.activation(out=dst[:], in_=tmpA[:], func=AF.Sin), reads=["tmpA"],
                     writes=[dname])
            else:
                P.op("act", lambda e: e.activation(out=dst[:], in_=tmpA[:], func=AF.Sin, scale=scale_ap),
                     reads=["tmpA", "sgn"], writes=[dname])

        range_reduce_sin(sinT, 0.0, sgn[:, 0:1], "sinT")
        range_reduce_sin(cosT, PI / 2, None, "cosT")
        P.op("pool", lambda e: e.iota(out=ci[:], pattern=[[1, 128]], base=1, channel_multiplier=0), writes=["ci"])
        P.op("dve", lambda e: e.tensor_copy(out=cf[:], in_=ci[:]), reads=["ci"], writes=["cf"])
        P.op("pool", lambda e: e.iota(out=pi_[:], pattern=[[0, 1]], base=1, channel_multiplier=1), writes=["pi_"])
        P.op("dve", lambda e: e.tensor_copy(out=pf[:], in_=pi_[:]), reads=["pi_"], writes=["pf"])
        for h in range(4):
            P.op("pool", lambda e, h=h: e.memset(biasc[:, h:h + 1], math.log(DKS)), writes=["biasc"])
            P.op("pool", lambda e, h=h: e.memset(biasc[:, 4 + h:5 + h], 128.0 * LNG[h] + math.log(DKS)),
                 writes=["biasc"])
        for h in range(4):
            P.op("act", lambda e, h=h: e.activation(out=qdec[:, h, :], in_=cf[:], func=AF.Exp, scale=LNG[h]),
                 reads=["cf"], writes=["qdec"])
            P.op("act", lambda e, h=h: e.activation(out=kfac[:, h:h + 1], in_=pf[:], func=AF.Exp, scale=-LNG[h],
                                                    bias=biasc[:, h:h + 1]),
                 reads=["pf", "biasc"], writes=["kfac"])
            P.op("act", lambda e, h=h: e.activation(out=kdec[:, h:h + 1], in_=pf[:], func=AF.Exp, scale=-LNG[h],
                                                    bias=biasc[:, 4 + h:5 + h]),
                 reads=["pf", "biasc"], writes=["kdec"])
        P.op("pool", lambda e: e.affine_select(out=mask01[:], in_=ones_f[:], pattern=[[1, 128]],
                                               compare_op=ALU.is_ge, fill=0.0, base=0, channel_multiplier=-1),
             reads=["ones_f"], writes=["mask01"])

        T_.update(cosT=cosT, sinT=sinT, qdec_done=True, mT=mT, end=P.sb_off)

    if _LAYOUT.get("tbl") is not None:
        _save = P.sb_off
        P.sb_off = _LAYOUT["tbl_small"]
        qdec = P.sb("qdec", [128, 4, 128], F32)
        kfac = P.sb("kfac", [128, 4], F32)
        kdec = P.sb("kdec", [128, 4], F32)
        mask01 = P.sb("mask01", [128, 128], F32)
        assert P.sb_off == _LAYOUT["tbl"]
        _tbl_early = True
        P.sb_off = _save
    else:
        _tbl_early = False

    mA = P.mark()
    cT = P.sb("cT", [128, KC], F32)
    sc = P.sb("sc", [128, KC], F32)
    mod_row = P.sb("mod_row", [1, 6 * D], F32)
    brow = P.sb("brow", [1, 6 * D], F32)
    grow = P.sb("grow", [1, D], F32)
    a2row = P.sb("a2row", [1, D], F32)
    wa = [P.sb(f"wa{i}", [128, KC, 512], BF16) for i in range(4)]
    sc_bf = P.sb("sc_bf", [128, KC], BF16)

    P.op("sp", lambda e: e.dma_start(out=cT[:], in_=cT_d), writes=["cT"], dma="c0")
    P.op("sp", lambda e: e.dma_start(out=brow[:], in_=b_ada_d), writes=["brow"], dma="c1")
    P.op("sp", lambda e: e.dma_start(out=gmix[:], in_=gmix_d), writes=["gmix"], dma="c2")
    P.op("sp", lambda e: e.dma_start(out=gffn[:], in_=gffn_d), writes=["gffn"], dma="c3")
    P.op("sp", lambda e: e.dma_start(out=grow[:], in_=gffn_r_d), writes=["grow"], dma="c4")
    P.op("act", lambda e: e.activation(out=sc[:], in_=cT[:], func=AF.Silu), reads=["cT"], writes=["sc"])
    P.op("act", lambda e: e.copy(out=sc_bf[:], in_=sc[:]), reads=["sc"], writes=["sc_bf"])
    for g in range(12):
        b = g % 4
        P.op("pool", lambda e, g=g, b=b: e.dma_start(
            out=wa[b][:], in_=w_ada_d[:, g * 512:(g + 1) * 512].rearrange("(k p) f -> p k f", p=128)),
            writes=[f"wa{b}"], dma=f"wa{b}")
        fns = []
        for k in range(KC):
            fns.append(lambda e, k=k, b=b: e.matmul(psf(b)[0:1, :], lhsT=sc_bf[:, k:k + 1], rhs=wa[b][:, k, :],
                                                   start=(k == 0), stop=(k == KC - 1)))
        P.group("pe", fns, reads=["sc_bf", f"wa{b}"], writes=[f"ps{b}"])
        P.op("dve", lambda e, g=g, b=b: e.tensor_tensor(out=mod_row[0:1, g * 512:(g + 1) * 512],
                                                        in0=psf(b)[0:1, :], in1=brow[0:1, g * 512:(g + 1) * 512],
                                                        op=ALU.add),
             reads=[f"ps{b}", "brow"], writes=["mod_row"])
        if g == 3 and _tbl_early:
            _save2 = P.sb_off
            P.sb_off = _LAYOUT["tbl"]
            build_tables()
            P.sb_off = _save2
    fns = []
    for j in range(48):
        fns.append(lambda e, j=j: e.matmul(psf(6)[:, j:j + 1], lhsT=mod_row[0:1, j * 128:(j + 1) * 128],
                                           rhs=ones_f[0:1, 0:1], start=True, stop=True))
    P.group("pe", fns, reads=["mod_row", "ones_f"], writes=["ps6"])
    P.op("dve", lambda e: e.tensor_copy(out=modT[:], in_=psf(6)[:, 0:48]), reads=["ps6"], writes=["modT"])
    P.op("dve", lambda e: e.scalar_tensor_tensor(out=A1[:], in0=modT[:, 8:16], scalar=1.0, in1=gmix[:],
                                                 op0=ALU.add, op1=ALU.mult),
         reads=["modT", "gmix"], writes=["A1"])
    P.op("dve", lambda e: e.scalar_tensor_tensor(out=A2[:], in0=modT[:, 32:40], scalar=1.0, in1=gffn[:],
                                                 op0=ALU.add, op1=ALU.mult),
         reads=["modT", "gffn"], writes=["A2"])
    P.op("dve", lambda e: e.scalar_tensor_tensor(out=a2row[:], in0=mod_row[0:1, 4 * D:5 * D], scalar=1.0,
                                                 in1=grow[:], op0=ALU.add, op1=ALU.mult),
         reads=["mod_row", "grow"], writes=["a2row"])
    srcs = [(mod_row, 2 * D), (mod_row, 5 * D), (a2row, 0), (mod_row, 3 * D)]
    for i, (src, off) in enumerate(srcs):
        for hh in range(2):
            b = 4 + (2 * i + hh) % 2
            P.op("pe", lambda e, src=src, off=off, hh=hh, b=b: e.matmul(
                psf(b)[:, :], lhsT=ones_f[0:1, :], rhs=src[0:1, off + hh * 512: off + (hh + 1) * 512],
                start=True, stop=True),
                reads=["mod_row", "a2row", "ones_f"], writes=[f"ps{b}"])
            P.op("act", lambda e, i=i, hh=hh, b=b: e.copy(out=bc[:, i, hh * 512:(hh + 1) * 512], in_=psf(b)[:, :]),
                 reads=[f"ps{b}"], writes=["bc"])
    d = dbg_out("modT", [128, 48])
    if d is not None:
        P.op("sp", lambda e, d=d: e.dma_start(out=d, in_=modT[:]), reads=["modT"], dma="dbg0")
        out_keys.append("dbg0")
    d = dbg_out("bc", [128, 4 * D])
    if d is not None:
        P.op("sp", lambda e, d=d: e.dma_start(out=d, in_=bc[:].rearrange("p a b -> p (a b)")), reads=["bc"], dma="dbg1")
        out_keys.append("dbg1")
    P.barrier()
    P.release(mA)


    if stop_after == "A":
        return finish()

    hT = P.sb("hT", [128, KC, S], BF16)
    cat_off = P.mark()
    concatT = P.sb("concatT", [128, KC, S], BF16)
    cat_end = P.mark()
    ssq = P.sb("ssq", [128, 64], F32)
    rt_ = P.sb("rt_", [128, 64], F32)
    rs_ = P.sb("rs_", [128, 64], F32)
    junk2 = P.sb("junk2", [128, 128], BF16)

    def load_win_group(buf, name, g):
        return P.op("pool", lambda e: e.dma_start(
            out=buf[:], in_=w_in_d[:, g * 512:(g + 1) * 512].rearrange("(k p) f -> p k f", p=128)),
            writes=[name], dma="d_" + name)

    mP1 = P.mark()
    xb = [P.sb(f"xb{i}", [128, D], F32) for i in range(2)]
    xn = [P.sb(f"xn{i}", [128, D], BF16) for i in range(2)]
    junk = P.sb("junk", [128, D], BF16)
    for i in range(NT):
        b = i % 2
        P.op("sp", lambda e, i=i, b=b: e.dma_start(out=xb[b][:], in_=x_d[i * 128:(i + 1) * 128, :]),
             writes=[f"xb{b}"], dma=f"d_xb{b}")
        P.op("act", lambda e, i=i, b=b: e.activation(out=junk[:], in_=xb[b][:], func=AF.Square,
                                                     accum_out=ssq[:, i:i + 1]),
             reads=[f"xb{b}"], writes=["junk", f"ssq{i}"])
        P.op("act", lambda e, i=i: e.activation(out=rt_[:, i:i + 1], in_=ssq[:, i:i + 1], func=AF.Sqrt,
                                                scale=1.0 / D, bias=epsb[:, 0:1]),
             reads=[f"ssq{i}", "epsb"], writes=[f"rt{i}"])
        P.op("dve", lambda e, i=i: e.reciprocal(out=rs_[:, i:i + 1], in_=rt_[:, i:i + 1]),
             reads=[f"rt{i}"], writes=[f"rs{i}"])
        P.op("act", lambda e, i=i, b=b: e.activation(out=xn[b][:], in_=xb[b][:], func=AF.Copy,
                                                     scale=rs_[:, i:i + 1]),
             reads=[f"xb{b}", f"rs{i}"], writes=[f"xn{b}"])
        pb = 5 + b
        fns = [lambda e, c=c, b=b, pb=pb: e.transpose(out=psh(pb)[:, c * 128:(c + 1) * 128],
                                                      in_=xn[b][:, c * 128:(c + 1) * 128], identity=ident_bf[:])
               for c in range(KC)]
        P.group("pe", fns, reads=[f"xn{b}", "ident_bf"], writes=[f"ps{pb}"])
        for c in range(KC):
            if c % 2 == 0:
                P.op("dve", lambda e, i=i, c=c, pb=pb: e.tensor_scalar(
                    out=hT[:, c, i * 128:(i + 1) * 128], in0=psh(pb)[:, c * 128:(c + 1) * 128],
                    scalar1=A1[:, c:c + 1], scalar2=modT[:, c:c + 1], op0=ALU.mult, op1=ALU.add),
                    reads=[f"ps{pb}", "A1", "modT"], writes=[f"hT{i}"])
            else:
                P.op("act", lambda e, i=i, c=c, pb=pb: e.activation(
                    out=hT[:, c, i * 128:(i + 1) * 128], in_=psh(pb)[:, c * 128:(c + 1) * 128],
                    func=AF.Identity, scale=A1[:, c:c + 1], bias=modT[:, c:c + 1]),
                    reads=[f"ps{pb}", "A1", "modT"], writes=[f"hT{i}"])
    P.release(mP1)
    hT_all = [f"hT{i}" for i in range(NT)]
    d = dbg_out("hT", [128, KC * S], BF16)
    if d is not None:
        P.op("sp", lambda e, d=d: e.dma_start(out=d, in_=hT[:].rearrange("p a b -> p (a b)")), reads=hT_all,
             dma="dbg2")
        out_keys.append("dbg2")
    if stop_after == "P1":
        return finish()


    import math
    mR = P.mark()
    LNG = [math.log1p(-2.0 ** (-5.0 - h)) for h in range(4)]
    DKS = 128.0 ** -0.5
    qT = P.sb("qT", [128, 4, S], BF16)
    kT = P.sb("kT", [128, 4, S], BF16)
    v_sb = P.sb("v_sb", [128, NT, 512], BF16)
    sg_sb = P.sb("sg_sb", [128, NT, 512], BF16)
    if _LAYOUT.get("tbl") is None:
        _LAYOUT["tbl_small_new"] = P.sb_off
        qdec = P.sb("qdec", [128, 4, 128], F32)
        kfac = P.sb("kfac", [128, 4], F32)
        kdec = P.sb("kdec", [128, 4], F32)
        mask01 = P.sb("mask01", [128, 128], F32)
    else:
        assert P.sb_off == _LAYOUT["tbl_small"], (P.sb_off, _LAYOUT["tbl_small"])
        P.sb_off = _LAYOUT["tbl"]
    mR2 = P.mark()
    if _LAYOUT.get("tbl") is None:
        _LAYOUT["tbl_new"] = P.sb_off
        build_tables()
    else:
        assert P.sb_off == _LAYOUT["tbl"], (P.sb_off, _LAYOUT["tbl"])
        P.sb_off = T_["end"]
    mT = T_["mT"]
    cosT = T_["cosT"]
    sinT = T_["sinT"]
    P.barrier()
    P.release(mT)

    wi = [P.sb(f"wi{i}", [128, KC, 512], BF16) for i in range(4)]
    rt1 = [P.sb(f"rt1_{i}", [128, 512], F32) for i in range(2)]
    rt2 = [P.sb(f"rt2_{i}", [128, 512], F32) for i in range(2)]
    for gi in range(4):
        load_win_group(wi[gi], f"wi{gi}", gi)
    cnt = 0
    for which in range(2):
        wa_, wp_ = wi[2 * which], wi[2 * which + 1]
        na_, np_ = f"wi{2 * which}", f"wi{2 * which + 1}"
        dstT = qT if which == 0 else kT
        dname = "qT" if which == 0 else "kT"
        for h in range(4):
            for tg in range(4):
                r = cnt % 2
                cnt += 1
                pa, pp = (0, 1) if r == 0 else (2, 3)
                hreads = [f"hT{i}" for i in range(4 * tg, 4 * tg + 4)]
                fns = [lambda e, k=k, h=h, tg=tg, pa=pa, wa_=wa_: e.matmul(
                    psf(pa)[:, :], lhsT=wa_[:, k, h * 128:(h + 1) * 128], rhs=hT[:, k, tg * 512:(tg + 1) * 512],
                    start=(k == 0), stop=(k == KC - 1)) for k in range(KC)]
                P.group("pe", fns, reads=hreads + [na_], writes=[f"ps{pa}"])
                fns = [lambda e, k=k, h=h, tg=tg, pp=pp, wp_=wp_: e.matmul(
                    psf(pp)[:, :], lhsT=wp_[:, k, h * 128:(h + 1) * 128], rhs=hT[:, k, tg * 512:(tg + 1) * 512],
                    start=(k == 0), stop=(k == KC - 1)) for k in range(KC)]
                P.group("pe", fns, reads=hreads + [np_], writes=[f"ps{pp}"])
                P.op("dve", lambda e, r=r, pa=pa, tg=tg: e.tensor_tensor(
                    out=rt1[r][:], in0=psf(pa)[:, :], in1=cosT[:, tg * 512:(tg + 1) * 512], op=ALU.mult),
                    reads=[f"ps{pa}", "cosT"], writes=[f"rt1_{r}"])
                P.op("dve", lambda e, r=r, pp=pp, tg=tg: e.tensor_tensor(
                    out=rt2[r][:], in0=psf(pp)[:, :], in1=sinT[:, tg * 512:(tg + 1) * 512], op=ALU.mult),
                    reads=[f"ps{pp}", "sinT"], writes=[f"rt2_{r}"])
                if which == 0:
                    P.op("pool", lambda e, r=r: e.tensor_tensor(out=rt1[r][:], in0=rt1[r][:], in1=rt2[r][:],
                                                                op=ALU.add),
                         reads=[f"rt1_{r}", f"rt2_{r}"], writes=[f"rt1_{r}"])
                    P.op("pool", lambda e, r=r, h=h, tg=tg: e.tensor_tensor(
                        out=qT[:, h, tg * 512:(tg + 1) * 512].rearrange("p (a b) -> p a b", b=128),
                        in0=rt1[r][:].rearrange("p (a b) -> p a b", b=128),
                        in1=qdec[:, h:h + 1, :].broadcast_to([128, 4, 128]), op=ALU.mult),
                        reads=[f"rt1_{r}", "qdec"], writes=[f"qT{h}_{tg}"])
                else:
                    P.op("pool", lambda e, r=r, h=h, tg=tg: e.tensor_tensor(
                        out=kT[:, h, tg * 512:(tg + 1) * 512], in0=rt1[r][:], in1=rt2[r][:], op=ALU.add),
                        reads=[f"rt1_{r}", f"rt2_{r}"], writes=[f"kT{h}_{tg}"])
    load_win_group(wi[0], "wi0", 4)
    load_win_group(wi[1], "wi1", 5)
    for which in range(2):
        w_ = wi[which]
        for i in range(NT):
            pb = 4 + (i % 2)
            fns = [lambda e, k=k, i=i, pb=pb, w_=w_: e.matmul(
                psf(pb)[:, :], lhsT=hT[:, k, i * 128:(i + 1) * 128], rhs=w_[:, k, :],
                start=(k == 0), stop=(k == KC - 1)) for k in range(KC)]
            P.group("pe", fns, reads=[f"hT{i}", f"wi{which}"], writes=[f"ps{pb}"])
            if which == 0:
                P.op("act", lambda e, i=i, pb=pb: e.copy(out=v_sb[:, i, :], in_=psf(pb)[:, :]),
                     reads=[f"ps{pb}"], writes=[f"v{i}"])
            else:
                P.op("act", lambda e, i=i, pb=pb: e.activation(out=sg_sb[:, i, :], in_=psf(pb)[:, :], func=AF.Silu),
                     reads=[f"ps{pb}"], writes=[f"sg{i}"])
    d = dbg_out("qT", [128, 4 * S], BF16)
    if d is not None:
        P.op("sp", lambda e, d=d: e.dma_start(out=d, in_=qT[:].rearrange("p a b -> p (a b)")),
             reads=[f"qT{h}_{tg}" for h in range(4) for tg in range(4)], dma="dbg3")
        out_keys.append("dbg3")
    d = dbg_out("kT", [128, 4 * S], BF16)
    if d is not None:
        P.op("sp", lambda e, d=d: e.dma_start(out=d, in_=kT[:].rearrange("p a b -> p (a b)")),
             reads=[f"kT{h}_{tg}" for h in range(4) for tg in range(4)], dma="dbg4")
        out_keys.append("dbg4")

    P.barrier()
    P.release(mR2)
    zt = P.sb("zt", [128, 4096], BF16)
    P.op("pool", lambda e: e.memset(zt[:], 0.0), writes=["zt"])
    for zi in range(32):
        P.op("sp", lambda e, zi=zi: e.dma_start(
            out=o8_d[zi * 512:(zi + 1) * 512, :].rearrange("(p a) d -> p (a d)", p=128), in_=zt[:]),
            reads=["zt"], dma="d_o8z")
    o8_init_tok = ("d_o8z", P.semval["d_o8z"])
    S_f = P.sb("S_f", [128, 4, 128], F32)
    S_bf = P.sb("S_bf", [128, 4, 128], BF16)
    ktok = [P.sb(f"ktok{i}", [128, 4, 128], BF16) for i in range(2)]
    PT = [P.sb(f"PT{i}", [128, 4, 128], BF16) for i in range(2)]
    ret_tok = [P.sb(f"ret_tok{i}", [128, 512], BF16) for i in range(2)]
    ssr = P.sb("ssr", [128, NT, 4], F32)
    rtr = P.sb("rtr", [128, NT, 4], F32)
    rsr = P.sb("rsr", [128, NT, 4], F32)
    CD = [math.exp(LNG[h] * 128.0) for h in range(4)]
    def rec_chunk(n):
        tg = n // 4
        r = n % 2
        csl = slice(n * 128, (n + 1) * 128)
        bSC, bO, bKV, bKT = 0 + r, 2 + r, 4 + r, 6 + r
        fns = [lambda e, h=h: e.matmul(psf(bSC)[:, h * 128:(h + 1) * 128], lhsT=kT[:, h, csl], rhs=qT[:, h, csl],
                                       start=True, stop=True) for h in range(4)]
        P.group("pe", fns, reads=[f"kT{h}_{tg}" for h in range(4)] + [f"qT{h}_{tg}" for h in range(4)],
                writes=[f"ps{bSC}"])
        if n < NT - 1:
            fns = [lambda e, h=h: e.transpose(out=psh(bKT)[:, h * 128:(h + 1) * 128], in_=kT[:, h, csl],
                                              identity=ident_bf[:]) for h in range(4)]
            P.group("pe", fns, reads=[f"kT{h}_{tg}" for h in range(4)] + ["ident_bf"], writes=[f"ps{bKT}a"])
        for h in range(4):
            P.op("dve", lambda e, h=h: e.scalar_tensor_tensor(
                out=PT[r][:, h, :], in0=psf(bSC)[:, h * 128:(h + 1) * 128], scalar=kfac[:, h:h + 1], in1=mask01[:],
                op0=ALU.mult, op1=ALU.mult), reads=[f"ps{bSC}", "kfac", "mask01"], writes=[f"PT{r}_{h}"])
        if n < NT - 1:
            for h in range(4):
                P.op("act", lambda e, h=h: e.activation(out=ktok[r][:, h, :], in_=psh(bKT)[:, h * 128:(h + 1) * 128],
                                                        func=AF.Copy, scale=kdec[:, h:h + 1]),
                     reads=[f"ps{bKT}a", "kdec"], writes=[f"ktok{r}_{h}"])
        for h in range(4):
            fns = [lambda e, h=h: e.matmul(psf(bO)[:, h * 128:(h + 1) * 128], lhsT=PT[r][:, h, :],
                                           rhs=v_sb[:, n, h * 128:(h + 1) * 128], start=True, stop=(n == 0))]
            rds = [f"PT{r}_{h}", f"v{n}"]
            if n > 0:
                fns.append(lambda e, h=h: e.matmul(psf(bO)[:, h * 128:(h + 1) * 128], lhsT=qT[:, h, csl],
                                                   rhs=S_bf[:, h, :], start=False, stop=True))
                rds += [f"qT{h}_{tg}", f"S_bf{h}"]
            P.group("pe", fns, reads=rds, writes=[f"ps{bO}"])
        if n < NT - 1:
            for h in range(4):
                P.op("pe", lambda e, h=h: e.matmul(psf(bKV)[:, h * 128:(h + 1) * 128], lhsT=ktok[r][:, h, :],
                                                   rhs=v_sb[:, n, h * 128:(h + 1) * 128], start=True, stop=True),
                     reads=[f"ktok{r}_{h}", f"v{n}"], writes=[f"ps{bKV}"])
            for h in range(4):
                if n == 0:
                    P.op("dve", lambda e, h=h: e.tensor_copy(out=S_f[:, h, :], in_=psf(bKV)[:, h * 128:(h + 1) * 128]),
                         reads=[f"ps{bKV}"], writes=[f"S_f{h}"])
                else:
                    P.op("dve", lambda e, h=h: e.scalar_tensor_tensor(
                        out=S_f[:, h, :], in0=S_f[:, h, :], scalar=CD[h], in1=psf(bKV)[:, h * 128:(h + 1) * 128],
                        op0=ALU.mult, op1=ALU.add), reads=[f"ps{bKV}", f"S_f{h}"], writes=[f"S_f{h}"])
                P.op("pool", lambda e, h=h: e.tensor_copy(out=S_bf[:, h, :], in_=S_f[:, h, :]),
                     reads=[f"S_f{h}"], writes=[f"S_bf{h}"])
        for h in range(4):
            P.op("act", lambda e, h=h: e.activation(out=junk2[:], in_=psf(bO)[:, h * 128:(h + 1) * 128],
                                                    func=AF.Square, accum_out=ssr[:, n, h:h + 1]),
                 reads=[f"ps{bO}"], writes=["junk2", f"ssr{n}"])
        P.op("act", lambda e: e.activation(out=rtr[:, n, :], in_=ssr[:, n, :], func=AF.Sqrt, scale=1.0 / 128.0,
                                           bias=epsb[:, 0:1]), reads=[f"ssr{n}", "epsb"], writes=[f"rtr{n}"])
        P.op("dve", lambda e: e.reciprocal(out=rsr[:, n, :], in_=rtr[:, n, :]), reads=[f"rtr{n}"],
             writes=[f"rsr{n}"])
        for h in range(4):
            P.op("dve", lambda e, h=h: e.scalar_tensor_tensor(
                out=ret_tok[r][:, h * 128:(h + 1) * 128], in0=psf(bO)[:, h * 128:(h + 1) * 128],
                scalar=rsr[:, n, h:h + 1], in1=sg_sb[:, n, h * 128:(h + 1) * 128], op0=ALU.mult, op1=ALU.mult),
                reads=[f"ps{bO}", f"rsr{n}", f"sg{n}"], writes=[f"ret_tok{r}"])
        fns = [lambda e, c=c: e.transpose(out=psh(bKT)[:, 512 + c * 128:512 + (c + 1) * 128],
                                          in_=ret_tok[r][:, c * 128:(c + 1) * 128], identity=ident_bf[:])
               for c in range(4)]
        P.group("pe", fns, reads=[f"ret_tok{r}", "ident_bf"], writes=[f"ps{bKT}b"])
        P.op("act", lambda e: e.copy(out=concatT[:, 0:4, n * 128:(n + 1) * 128],
                                     in_=psh(bKT)[:, 512:1024].rearrange("p (a b) -> p a b", b=128)),
             reads=[f"ps{bKT}b"], writes=[f"cat_r{n}"])

    for n_ in range(NT):
        rec_chunk(n_)
    cat_r = [f"cat_r{n}" for n in range(NT)]
    P.barrier()
    P.release(mR)
    if stop_after == "R":
        d = dbg_out("concatT", [128, KC * S], BF16)
        if d is not None:
            P.op("sp", lambda e, d=d: e.dma_start(out=d, in_=concatT[:].rearrange("p a b -> p (a b)")),
                 dma="dbg5")
            out_keys.append("dbg5")
        return finish()


    mM = P.mark()
    qnT = P.sb("qnT", [128, 4, S], BF16)
    knT = P.sb("knT", [128, 4, S], BF16)
    v_aug = P.sb("v_aug", [128, NT, 8, 128], BF16)
    kmeanT = P.sb("kmeanT", [128, 4, 8], BF16)
    kmtmp = P.sb("kmtmp", [128, 4, 8], F32)
    mask_bf = P.sb("mask_bf", [128, 128], BF16)
    negm = P.sb("negm", [128, 4, 8], F32)
    gain_q = P.sb("gain_q", [128, 64], F32)
    gain_k = P.sb("gain_k", [128, 64], F32)
    mM2 = P.mark()
    wim = [P.sb(f"wim{i}", [128, KC, 512], BF16) for i in range(3)]
    sqj = [P.sb(f"sqj{i}", [128, 512], F32) for i in range(2)]
    nrm1 = [P.sb(f"nrm1_{i}", [128, 512], F32) for i in range(2)]
    qk_tok = [P.sb(f"qk_tok{i}", [128, 512], BF16) for i in range(2)]
    ssm = P.sb("ssm", [128, 2 * NT, 8], F32)
    rtm = P.sb("rtm", [128, 2 * NT, 8], F32)
    rsm = P.sb("rsm", [128, 2 * NT, 8], F32)

    for gi in range(3):
        P.op("pool", lambda e, gi=gi: e.dma_start(
            out=wim[gi][:], in_=w_in_d[:, (6 + gi) * 512:(7 + gi) * 512].rearrange("(k p) f -> p k f", p=128)),
            writes=[f"wim{gi}"], dma=f"d_wim{gi}")
    grow2 = P.sb("grow2", [1, 128], F32)
    P.op("sp", lambda e: e.dma_start(out=grow2[0:1, 0:64], in_=qg_d), writes=["grow2a"], dma="c5")
    P.op("sp", lambda e: e.dma_start(out=grow2[0:1, 64:128], in_=kg_d), writes=["grow2b"], dma="c6")
    P.op("pe", lambda e: e.matmul(psf(7)[:, 0:128], lhsT=ones_f[0:1, :], rhs=grow2[0:1, :], start=True, stop=True),
         reads=["grow2a", "grow2b", "ones_f"], writes=["ps7"])
    P.op("dve", lambda e: e.tensor_copy(out=gain_q[:], in_=psf(7)[:, 0:64]), reads=["ps7"], writes=["gain_q"])
    P.op("dve", lambda e: e.tensor_copy(out=gain_k[:], in_=psf(7)[:, 64:128]), reads=["ps7"], writes=["gain_k"])
    P.op("pool", lambda e: e.memset(v_aug[:].rearrange("p a b c -> p (a b c)"), 1.0), writes=["v_aug"])
    P.op("pool", lambda e: e.affine_select(out=mask_bf[:], in_=ones_f[:], pattern=[[1, 128]],
                                           compare_op=ALU.is_ge, fill=0.0, base=0, channel_multiplier=-1),
         reads=["ones_f"], writes=["mask_bf"])
    P.op("pool", lambda e: e.memset(negm[:].rearrange("p a b -> p (a b)"), 0.0), writes=["negm"])
    for bb in range(4, 8):
        P.op("pool", lambda e, bb=bb: e.memset(negm[:, bb - 4, bb:8], -1.0e30), writes=["negm"])

    def m_mm(i):
        for which in range(3):
            pb = which + 3 * (i % 2)
            fns = [lambda e, k=k, pb=pb, which=which: e.matmul(
                psf(pb)[:, :], lhsT=hT[:, k, i * 128:(i + 1) * 128], rhs=wim[which][:, k, :],
                start=(k == 0), stop=(k == KC - 1)) for k in range(KC)]
            P.group("pe", fns, reads=[f"hT{i}", f"wim{which}"], writes=[f"ps{pb}"])

    def m_post(i):
        pbq, pbk, pbv = 0 + 3 * (i % 2), 1 + 3 * (i % 2), 2 + 3 * (i % 2)
        pbs = [pbq, pbk]
        cols = [2 * i, 2 * i + 1]
        gains = [gain_q, gain_k]
        gnames = ["gain_q", "gain_k"]
        for w in range(2):
            P.op("act", lambda e, w=w: e.activation(out=sqj[w][:], in_=psf(pbs[w])[:, :], func=AF.Square),
                 reads=[f"ps{pbs[w]}"], writes=[f"sqj{w}"])
        P.op("act", lambda e: e.copy(
            out=v_aug[:, i, 0::2, 0:64],
            in_=psf(pbv)[:, :].rearrange("p (a u d) -> p a u d", u=2, d=64)[:, :, 0, :]),
            reads=[f"ps{pbv}"], writes=[f"va{i}"], extra=[v_aug_tok])
        P.op("act", lambda e: e.copy(
            out=v_aug[:, i, 1::2, 64:128],
            in_=psf(pbv)[:, :].rearrange("p (a u d) -> p a u d", u=2, d=64)[:, :, 1, :]),
            reads=[f"ps{pbv}"], writes=[f"va{i}"], extra=[v_aug_tok])
        for w in range(2):
            P.op("dve", lambda e, w=w: e.tensor_reduce(
                out=ssm[:, cols[w], :], in_=sqj[w][:].rearrange("p (h d) -> p h d", d=64), axis=AX.X, op=ALU.add),
                reads=[f"sqj{w}"], writes=[f"ssm{cols[w]}"])
        for w in range(2):
            P.op("act", lambda e, w=w: e.activation(out=rtm[:, cols[w], :], in_=ssm[:, cols[w], :], func=AF.Sqrt,
                                                    scale=1.0 / 64.0, bias=epsb[:, 0:1]),
                 reads=[f"ssm{cols[w]}", "epsb"], writes=[f"rtm{cols[w]}"])
        for w in range(2):
            P.op("dve", lambda e, w=w: e.reciprocal(out=rsm[:, cols[w], :], in_=rtm[:, cols[w], :]),
                 reads=[f"rtm{cols[w]}"], writes=[f"rsm{cols[w]}"])
        for w in range(2):
            P.op("dve", lambda e, w=w: e.tensor_tensor(
                out=nrm1[w][:].rearrange("p (h d) -> p h d", d=64),
                in0=psf(pbs[w])[:, :].rearrange("p (h d) -> p h d", d=64),
                in1=rsm[:, cols[w], :].unsqueeze(2).broadcast_to([128, 8, 64]), op=ALU.mult),
                reads=[f"ps{pbs[w]}", f"rsm{cols[w]}"], writes=[f"nrm1_{w}"])
        for w in range(2):
            P.op("pool", lambda e, w=w: e.tensor_tensor(
                out=qk_tok[w][:].rearrange("p (h d) -> p h d", d=64),
                in0=nrm1[w][:].rearrange("p (h d) -> p h d", d=64),
                in1=gains[w][:].unsqueeze(1).broadcast_to([128, 8, 64]), op=ALU.mult),
                reads=[f"nrm1_{w}", gnames[w]], writes=[f"qk_tok{w}"])
        for w in range(2):
            to = w * 512
            fns = [lambda e, c=c, w=w, to=to: e.transpose(out=psh(6)[:, to + c * 128:to + (c + 1) * 128],
                                                          in_=qk_tok[w][:, c * 128:(c + 1) * 128],
                                                          identity=ident_bf[:]) for c in range(4)]
            P.group("pe", fns, reads=[f"qk_tok{w}", "ident_bf"], writes=["ps6"])
        fns = [lambda e, c=c: e.matmul(psf(7)[:, c * 16 + i:c * 16 + i + 1],
                                       lhsT=qk_tok[1][:, c * 128:(c + 1) * 128], rhs=ones_bf[:, 0:1],
                                       start=True, stop=True) for c in range(4)]
        P.group("pe", fns, reads=["qk_tok1", "ones_bf"], writes=["ps7"])
        for w in range(2):
            to = w * 512
            dstT = qnT if w == 0 else knT
            P.op("act", lambda e, to=to, dstT=dstT: e.copy(
                out=dstT[:, :, i * 128:(i + 1) * 128],
                in_=psh(6)[:, to:to + 512].rearrange("p (a b) -> p a b", b=128)),
                reads=["ps6"], writes=[("qn" if w == 0 else "kn") + str(i)])

    v_aug_tok = P.last_w.get("v_aug")
    m_mm(0)
    for i in range(NT):
        if i + 1 < NT:
            m_mm(i + 1)
        m_post(i)
    P.op("dve", lambda e: e.tensor_tensor(
        out=kmtmp[:], in0=psf(7)[:, 0:64].rearrange("p (c n t) -> p c n t", c=4, t=2)[:, :, :, 0],
        in1=ones_f[:, 0:32].rearrange("p (c n) -> p c n", c=4), op=ALU.mult),
        reads=["ps7", "ones_f"], writes=["kmtmp"])
    P.op("dve", lambda e: e.tensor_tensor(
        out=kmtmp[:], in0=psf(7)[:, 0:64].rearrange("p (c n t) -> p c n t", c=4, t=2)[:, :, :, 1],
        in1=kmtmp[:], op=ALU.add),
        reads=["ps7", "kmtmp"], writes=["kmtmp"])
    P.op("dve", lambda e: e.tensor_scalar(out=kmeanT[:], in0=kmtmp[:], scalar1=1.0 / 256.0, scalar2=None,
                                          op0=ALU.mult), reads=["kmtmp"], writes=["kmeanT"])
    P.barrier()
    if stop_after == "M1":
        d = dbg_out("qnT", [128, 4 * S], BF16)
        if d is not None:
            P.op("sp", lambda e, d=d: e.dma_start(out=d, in_=qnT[:].rearrange("p a b -> p (a b)")), dma="dbg6")
            out_keys.append("dbg6")
        return finish()
    P.release(mM2)
    gm = P.sb("gm", [128, 8, 8], F32)
    m8 = P.sb("m8", [128, 8, 8], F32)
    bsel = P.sb("bsel", [128, 8, 8], F32)
    bsel_bf = P.sb("bsel_bf", [128, 2, 64], BF16)
    pexp = [P.sb(f"pexp{i}", [128, 256], BF16) for i in range(4)]
    rden = [P.sb(f"rden{i}", [128, 256], F32) for i in range(2)]
    biasT8s = [P.sb(f"biasT8_{i}", [128, 256], BF16) for i in range(2)]
    ind64 = P.sb("ind64", [128, 64, 128], BF16)
    P.op("pool", lambda e: e.memset(ind64[:].rearrange("p a b -> p (a b)"), 1.0), writes=["ind64"])
    for hf in range(2):
        P.op("pool", lambda e, hf=hf: e.affine_select(
            out=ind64[hf * 64:(hf + 1) * 64].rearrange("p a b -> p (a b)"),
            in_=ind64[hf * 64:(hf + 1) * 64].rearrange("p a b -> p (a b)"),
            pattern=[[-1, 64], [0, 128]], compare_op=ALU.is_equal, fill=0.0, base=0, channel_multiplier=1),
            reads=["ind64"], writes=["ind64"])

    def emit_bias(i):
        bb = i // 2
        biasT8 = biasT8s[bb % 2]
        bname = f"biasT8_{bb % 2}"
        io = (i % 2) * 128
        fns = [lambda e, h=h, i=i: e.matmul(
            psf(4 if h % 2 == 0 else 7)[:, (h // 2) * 8:(h // 2 + 1) * 8],
            lhsT=qnT[(h % 2) * 64:(h % 2 + 1) * 64, h // 2, i * 128:(i + 1) * 128],
            rhs=kmeanT[(h % 2) * 64:(h % 2 + 1) * 64, h // 2, :], start=True, stop=True) for h in range(8)]
        P.group("pe", fns, writes=["ps4", "ps7"])
        if BIS < 2:
            return
        for par, bk in ((0, 4), (1, 7)):
            P.op("dve", lambda e, bb=bb, par=par, bk=bk: e.tensor_tensor(
                out=gm[:, par::2, :], in0=psf(bk)[:, 0:32].rearrange("p (h n) -> p h n", n=8),
                in1=negm[:, bb - 4:bb - 3, :].broadcast_to([128, 4, 8]), op=ALU.add),
                reads=[f"ps{bk}"], writes=["gm"])
        if BIS < 3:
            return
        for h in range(8):
            P.op("dve", lambda e, h=h: e.max(out=m8[:, h, :], in_=gm[:, h, :]), reads=["gm"], writes=["m8"])
        if BIS < 4:
            return
        P.op("dve", lambda e: e.tensor_tensor(out=bsel[:], in0=gm[:], in1=m8[:, :, 2:3].broadcast_to([128, 8, 8]),
                                              op=ALU.is_lt), reads=["gm", "m8"], writes=["bsel"])
        if BIS < 5:
            return
        for dup in range(2):
            P.op("dve", lambda e, dup=dup: e.tensor_scalar(
                out=bsel_bf[:, dup, :], in0=bsel[:].rearrange("p a b -> p (a b)"), scalar1=-30000.0, scalar2=None,
                op0=ALU.mult), reads=["bsel"], writes=["bsel_bf"])
        if BIS < 6:
            return
        P.op("pe", lambda e: e.transpose(out=psh(4)[:, 512:640], in_=bsel_bf[:].rearrange("p a b -> p (a b)"),
                                         identity=ident_bf[:]), reads=["bsel_bf", "ident_bf"], writes=["ps4"])
        if BIS < 7:
            return
        P.op("act", lambda e, io=io, biasT8=biasT8: e.copy(out=biasT8[:, io:io + 128], in_=psh(4)[:, 512:640]),
             reads=["ps4"], writes=[bname])

    if stop_after == "M2":
        emit_bias(8)
        emit_bias(9)
        P.barrier()
        return finish()
    lnd = [P.sb(f"lnd{i}", [128, 256], F32) for i in range(2)]
    tasks = []
    for bb in range(8 if stop_after != "M3" else 1):
        for j in range(4):
            for u in range(2):
                chunks = []
                for n in range(bb):
                    chunks.append((n, 2 * n, "past"))
                    chunks.append((n, 2 * n + 1, "past"))
                chunks.append((bb, 2 * bb, "own0"))
                chunks.append((bb, 2 * bb + 1, "own1"))
                for ci_, (n, kt, kind) in enumerate(chunks):
                    tasks.append(dict(bb=bb, j=j, u=u, h=2 * j + u, n=n, kt=kt, kind=kind, ci=ci_,
                                      last=(ci_ == len(chunks) - 1), first_of_block=(j == 0 and u == 0 and ci_ == 0)))
    SCB = [0, 1, 6, 5]
    LA = 3
    ocnt_map = {}
    oc = 0
    for t_ in tasks:
        key_ = (t_["bb"], t_["h"])
        if key_ not in ocnt_map:
            ocnt_map[key_] = oc
            oc += 1

    def emit_qk(tix, tk):
        bb, j, u, h, n, kt, kind = tk["bb"], tk["j"], tk["u"], tk["h"], tk["n"], tk["kt"], tk["kind"]
        if tk["first_of_block"] and bb >= 4:
            emit_bias(2 * bb)
            emit_bias(2 * bb + 1)
        biasT8 = biasT8s[bb % 2]
        bname = f"biasT8_{bb % 2}"
        pr = slice(u * 64, (u + 1) * 64)
        q0 = bb * 256
        sb_ = SCB[tix % 4]
        ks = slice(kt * 128, (kt + 1) * 128)
        if kind == "own1":
            qs = slice(q0 + 128, q0 + 256)
            bs = slice(128, 256)
            nq = 128
        else:
            qs = slice(q0, q0 + 256)
            bs = slice(0, 256)
            nq = 256
        use_bias = (kind == "past" and bb >= 4)
        fns = [lambda e: e.matmul(psf(sb_)[:, 0:nq], lhsT=knT[pr, j, ks], rhs=qnT[pr, j, qs], start=True,
                                  stop=(not use_bias))]
        rds = []
        if use_bias:
            fns.append(lambda e: e.matmul(psf(sb_)[:, 0:nq], lhsT=ind64[pr, h * 8 + n, :], rhs=biasT8[pr, bs],
                                          start=False, stop=True))
            rds = [bname, "ind64"]
        P.group("pe", fns, reads=rds, writes=[f"ps{sb_}"])

    def emit_pv(tix, tk):
        bb, j, u, h, n, kt, kind = tk["bb"], tk["j"], tk["u"], tk["h"], tk["n"], tk["kt"], tk["kind"]
        pr = slice(u * 64, (u + 1) * 64)
        dr = slice((1 - u) * 64, (2 - u) * 64)
        q0 = bb * 256
        sb_ = SCB[tix % 4]
        pxb = tix % 4
        oc_ = ocnt_map[(bb, h)]
        ob = 2 + (oc_ % 2)
        nq = 128 if kind == "own1" else 256
        P.op("act", lambda e: e.activation(out=pexp[pxb][:, 0:nq], in_=psf(sb_)[:, 0:nq], func=AF.Exp, scale=0.125),
             reads=[f"ps{sb_}"], writes=[f"pexp{pxb}"])
        if kind in ("own0", "own1"):
            P.op("pool", lambda e: e.tensor_tensor(out=pexp[pxb][:, 0:128], in0=pexp[pxb][:, 0:128], in1=mask_bf[:],
                                                   op=ALU.mult),
                 reads=[f"pexp{pxb}", "mask_bf"], writes=[f"pexp{pxb}"])
        oreg = psf(ob)[:, 128:256] if kind == "own1" else psf(ob)[:, 0:256]
        P.op("pe", lambda e: e.matmul(oreg, lhsT=v_aug[:, kt, h, :], rhs=pexp[pxb][:, 0:nq], start=(tk["ci"] == 0),
                                      stop=tk["last"]),
             reads=[f"pexp{pxb}", f"va{kt}"], writes=[f"ps{ob}"])
        if tk["last"]:
            rb_ = oc_ % 2
            P.op("act", lambda e: e.activation(out=lnd[rb_][pr, :], in_=psf(ob)[dr, 0:256], func=AF.Ln),
                 reads=[f"ps{ob}"], writes=[f"lnd{rb_}"])
            P.op("act", lambda e: e.activation(out=rden[rb_][pr, :], in_=lnd[rb_][pr, :], func=AF.Exp, scale=-1.0),
                 reads=[f"lnd{rb_}"], writes=[f"rden{rb_}"])
            P.op("dve", lambda e: e.tensor_tensor(out=concatT[pr, 4 + j, q0:q0 + 256], in0=psf(ob)[pr, 0:256],
                                                  in1=rden[rb_][pr, :], op=ALU.mult),
                 reads=[f"ps{ob}", f"rden{rb_}"], writes=[f"cat_m{h}_{bb}"])

    for tix in range(len(tasks) + LA):
        if tix < len(tasks):
            emit_qk(tix, tasks[tix])
        if tix - LA >= 0:
            emit_pv(tix - LA, tasks[tix - LA])
    P.barrier()
    P.release(mM)
    if stop_after == "M":
        d = dbg_out("concatT", [128, KC * S], BF16)
        if d is not None:
            P.op("sp", lambda e, d=d: e.dma_start(out=d, in_=concatT[:].rearrange("p a b -> p (a b)")),
                 dma="dbg5")
            out_keys.append("dbg5")
        return finish()


    xres = P.sb("xres", [128, NT, D], F32)
    mO = P.mark()
    wo = P.sb("wo", [128, KC, D], BF16)
    P.op("pool", lambda e: e.dma_start(out=wo[:], in_=w_out_d.rearrange("(k p) f -> p k f", p=128)),
         writes=["wo"], dma="d_wo")
    for i in range(NT):
        P.op("sp", lambda e, i=i: e.dma_start(out=xres[:, i, :], in_=x_d[i * 128:(i + 1) * 128, :]),
             writes=[f"xres{i}"], dma="d_xres")
    for i in range(NT):
        P.last_w[f"xres{i}"] = ("d_xres", P.semval["d_xres"])
    for k in range(KC):
        eng = "dve" if k % 2 == 0 else "pool"
        P.op(eng, lambda e, k=k: e.tensor_tensor(out=wo[:, k, :], in0=wo[:, k, :], in1=bc[:, 0, :], op=ALU.mult),
             reads=["wo", "bc"], writes=["wo"])
    for i in range(NT):
        for hh in range(2):
            pb = (2 * i + hh) % 4
            fns = [lambda e, c=c, i=i, hh=hh, pb=pb: e.matmul(
                psf(pb)[:, :], lhsT=concatT[:, c, i * 128:(i + 1) * 128], rhs=wo[:, c, hh * 512:(hh + 1) * 512],
                start=(c == 0), stop=(c == KC - 1)) for c in range(KC)]
            P.group("pe", fns, reads=["wo"], writes=[f"ps{pb}"])
            P.op("dve", lambda e, i=i, hh=hh, pb=pb: e.tensor_tensor(
                out=xres[:, i, hh * 512:(hh + 1) * 512], in0=psf(pb)[:, :], in1=xres[:, i, hh * 512:(hh + 1) * 512],
                op=ALU.add), reads=[f"ps{pb}", f"xres{i}"], writes=[f"xres{i}"])
    d = dbg_out("x1", [S, D])
    if d is not None:
        for i in range(NT):
            P.op("sp", lambda e, d=d, i=i: e.dma_start(out=d[i * 128:(i + 1) * 128, :], in_=xres[:, i, :]),
                 reads=[f"xres{i}"], dma="dbg7")
        out_keys.append("dbg7")
    P.barrier()
    P.release(mO)
    if stop_after == "O":
        return finish()

    dbg_out("Wd", [S, NEXP])
    Mall = P.sb("Mall", [128, NT, NEXP], BF16)
    dest8 = P.sb("dest8", [128, NT, 8], I32)
    W8 = P.sb("W8", [128, NT, 8], F32)
    mF = P.mark()
    wr = P.sb("wr", [128, KC, NEXP], F32)
    rb_row = P.sb("rb_row", [1, NEXP], F32)
    rb_bc = P.sb("rb_bc", [128, NEXP], F32)
    Ltri = P.sb("Ltri", [128, 128], BF16)
    base_e = P.sb("base_e", [128, NEXP], F32)
    base_i = P.sb("base_i", [128, NEXP], I32)
    cnt_bc = P.sb("cnt_bc", [128, NEXP], F32)
    junkF = P.sb("junkF", [128, D], BF16)
    h2f = [P.sb(f"h2f{i}", [128, D], F32) for i in range(2)]
    h2b = [P.sb(f"h2b{i}", [128, D], BF16) for i in range(2)]
    h2fT = P.sb("h2fT", [128, KC, 128], F32)
    sc_t2 = [P.sb(f"sc_t{i}", [128, NEXP], F32) for i in range(2)]
    biased = P.sb("biased", [128, NEXP], F32)
    choice = P.sb("choice", [128, NEXP], F32)
    Mf = P.sb("Mf", [128, NEXP], F32)
    ws_t = P.sb("ws_t", [128, NEXP], F32)
    Wd = P.sb("Wd", [128, NEXP], F32)
    keyt = P.sb("keyt", [128, NEXP], F32)
    jk = P.sb("jk", [128, NEXP], F32)
    g8 = P.sb("g8", [128, 8, 8], F32)
    gs = P.sb("gs", [128, 8], F32)
    gs8 = P.sb("gs8", [128, 8], F32)
    pen = P.sb("pen", [128, 8], F32)
    t8 = P.sb("t8", [128, 8], F32)
    key8 = P.sb("key8", [128, 8], F32)
    sm = P.sb("sm", [128, 4 * NT], F32)

    P.op("sp", lambda e: e.dma_start(out=wr[:], in_=w_router_d.rearrange("(k p) f -> p k f", p=128)),
         writes=["wr"], dma="c7")
    P.op("sp", lambda e: e.dma_start(out=rb_row[:], in_=rbias_d), writes=["rb_row"], dma="c8")
    P.op("pe", lambda e: e.matmul(psf(7)[:, 0:NEXP], lhsT=ones_f[0:1, :], rhs=rb_row[0:1, :], start=True, stop=True),
         reads=["rb_row", "ones_f"], writes=["ps7"])
    P.op("dve", lambda e: e.tensor_copy(out=rb_bc[:], in_=psf(7)[:, 0:NEXP]), reads=["ps7"], writes=["rb_bc"])
    P.op("pool", lambda e: e.affine_select(out=Ltri[:], in_=ones_f[:], pattern=[[1, 128]], compare_op=ALU.is_gt,
                                           fill=0.0, base=0, channel_multiplier=-1),
         reads=["ones_f"], writes=["Ltri"])
    P.op("pool", lambda e: e.iota(out=base_i[:], pattern=[[CAP, NEXP]], base=1, channel_multiplier=0),
         writes=["base_i"])
    P.op("dve", lambda e: e.tensor_copy(out=base_e[:], in_=base_i[:]), reads=["base_i"], writes=["base_e"])
    P.op("pool", lambda e: e.memset(cnt_bc[:], 0.0), writes=["cnt_bc"])
    bigi = P.sb("bigi", [128, 1024], I32)
    tokid = P.sb("tokid", [128, NT, 8, 2], I32)
    P.op("pool", lambda e: e.iota(out=bigi[:], pattern=[[0, 1024]], base=1 << 20, channel_multiplier=0),
         writes=["bigi"])
    tok_init = P.op("sp", lambda e: e.dma_start(out=tokidx_d.rearrange("(p f) o -> p (f o)", p=128), in_=bigi[:]),
                    reads=["bigi"], dma="c12")
    P.op("pool", lambda e: e.iota(out=tokid[:, :, :, 0], pattern=[[128, NT], [0, 8]], base=0,
                                  channel_multiplier=1), writes=["tokid"])
    P.op("pool", lambda e: e.iota(out=tokid[:, :, :, 1], pattern=[[1024, NT], [1, 8]], base=0,
                                  channel_multiplier=8), writes=["tokid"])

    def f_stageA(i):
        b = i % 2
        xr = f"xres{i}"
        P.op("act", lambda e, i=i: e.activation(out=junkF[:], in_=xres[:, i, :], func=AF.Square,
                                                accum_out=sm[:, 4 * i:4 * i + 1]),
             reads=[xr], writes=["junkF", f"sm{i}a"])
        P.op("act", lambda e, i=i: e.activation(out=sm[:, 4 * i + 1:4 * i + 2], in_=sm[:, 4 * i:4 * i + 1],
                                                func=AF.Sqrt, scale=1.0 / D, bias=epsb[:, 0:1]),
             reads=[f"sm{i}a", "epsb"], writes=[f"sm{i}b"])
        P.op("dve", lambda e, i=i: e.reciprocal(out=sm[:, 4 * i + 2:4 * i + 3], in_=sm[:, 4 * i + 1:4 * i + 2]),
             reads=[f"sm{i}b"], writes=[f"sm{i}c"])
        P.op("dve", lambda e, i=i, b=b: e.scalar_tensor_tensor(
            out=h2f[b][:], in0=xres[:, i, :], scalar=sm[:, 4 * i + 2:4 * i + 3], in1=bc[:, 2, :], op0=ALU.mult,
            op1=ALU.mult), reads=[xr, f"sm{i}c", "bc"], writes=[f"h2f{b}"])
        P.op("pool", lambda e, b=b: e.tensor_tensor(out=h2f[b][:], in0=h2f[b][:], in1=bc[:, 3, :], op=ALU.add),
             reads=[f"h2f{b}", "bc"], writes=[f"h2f{b}"])
        P.op("act", lambda e, b=b: e.copy(out=h2b[b][:], in_=h2f[b][:]), reads=[f"h2f{b}"], writes=[f"h2b{b}"])
        fns = [lambda e, c=c, b=b: e.transpose(out=psh(0)[:, c * 128:(c + 1) * 128],
                                               in_=h2b[b][:, c * 128:(c + 1) * 128], identity=ident_bf[:])
               for c in range(KC)]
        P.group("pe", fns, reads=[f"h2b{b}", "ident_bf"], writes=["ps0"])
        P.op("act", lambda e, i=i: e.copy(out=hT[:, :, i * 128:(i + 1) * 128],
                                          in_=psh(0)[:, :].rearrange("p (a b) -> p a b", b=128)),
             reads=["ps0"], writes=[f"hT{i}"])
        for half in range(2):
            fns = [lambda e, c=c, b=b, half=half: e.transpose(
                out=psf(1 + half)[:, (c % 4) * 128:(c % 4 + 1) * 128], in_=h2f[b][:, c * 128:(c + 1) * 128],
                identity=ident_f[:]) for c in range(4 * half, 4 * half + 4)]
            P.group("pe", fns, reads=[f"h2f{b}", "ident_f"], writes=[f"ps{1 + half}"])
            P.op("act", lambda e, half=half: e.copy(
                out=h2fT[:, 4 * half:4 * half + 4, :],
                in_=psf(1 + half)[:, :].rearrange("p (a b) -> p a b", b=128)),
                reads=[f"ps{1 + half}"], writes=[f"h2fT{half}"])
        fns = [lambda e, c=c: e.matmul(psf(3)[:, 0:NEXP], lhsT=h2fT[:, c, :], rhs=wr[:, c, :], start=(c == 0),
                                       stop=(c == KC - 1)) for c in range(KC)]
        P.group("pe", fns, reads=["h2fT0", "h2fT1", "wr"], writes=["ps3"])
        P.op("act", lambda e: e.activation(out=sc_t2[i % 2][:], in_=psf(3)[:, 0:NEXP], func=AF.Sigmoid), reads=["ps3"],
             writes=[f"sc_t{i % 2}"])

    def f_stageB(i):
        b = i % 2
        P.op("dve", lambda e: e.tensor_tensor(out=biased[:], in0=sc_t2[i % 2][:], in1=rb_bc[:], op=ALU.add),
             reads=[f"sc_t{i % 2}", "rb_bc"], writes=["biased"])
        for g in range(8):
            P.op("dve", lambda e, g=g: e.max(out=g8[:, g, :], in_=biased[:, g * 32:(g + 1) * 32]),
                 reads=["biased"], writes=["g8"])
        P.op("dve", lambda e: e.tensor_tensor(out=gs[:], in0=g8[:, :, 0], in1=g8[:, :, 1], op=ALU.add),
             reads=["g8"], writes=["gs"])
        P.op("dve", lambda e: e.max(out=gs8[:], in_=gs[:]), reads=["gs"], writes=["gs8"])
        P.op("dve", lambda e: e.tensor_scalar(out=pen[:], in0=gs[:], scalar1=gs8[:, 3:4], scalar2=-1.0e9,
                                              op0=ALU.is_lt, op1=ALU.mult), reads=["gs", "gs8"], writes=["pen"])
        P.op("dve", lambda e: e.tensor_tensor(out=choice[:].rearrange("p (g j) -> p g j", j=32),
                                              in0=biased[:].rearrange("p (g j) -> p g j", j=32),
                                              in1=pen[:].unsqueeze(2).broadcast_to([128, 8, 32]), op=ALU.add),
             reads=["biased", "pen"], writes=["choice"])
        P.op("dve", lambda e: e.max(out=t8[:], in_=choice[:]), reads=["choice"], writes=["t8"])
        P.op("dve", lambda e: e.tensor_scalar(out=Mf[:], in0=choice[:], scalar1=t8[:, 7:8], scalar2=None,
                                              op0=ALU.is_ge), reads=["choice", "t8"], writes=["Mf"])
        P.op("pool", lambda e, i=i: e.tensor_copy(out=Mall[:, i, :], in_=Mf[:]), reads=["Mf"], writes=[f"Mall{i}"])
        P.op("dve", lambda e: e.tensor_tensor(out=ws_t[:], in0=sc_t2[i % 2][:], in1=Mf[:], op=ALU.mult),
             reads=[f"sc_t{i % 2}", "Mf"], writes=["ws_t"])
        P.op("dve", lambda e, i=i: e.tensor_reduce(out=sm[:, 4 * i + 3:4 * i + 4], in_=ws_t[:], axis=AX.X,
                                                   op=ALU.add), reads=["ws_t"], writes=[f"sm{i}d"])
        P.op("dve", lambda e, i=i: e.reciprocal(out=sm[:, 4 * i + 3:4 * i + 4], in_=sm[:, 4 * i + 3:4 * i + 4]),
             reads=[f"sm{i}d"], writes=[f"sm{i}d"])
        P.op("dve", lambda e, i=i: e.tensor_scalar(out=Wd[:], in0=ws_t[:], scalar1=sm[:, 4 * i + 3:4 * i + 4],
                                                   scalar2=2.5, op0=ALU.mult, op1=ALU.mult),
             reads=["ws_t", f"sm{i}d"], writes=["Wd"])
        d = dbg.get("Wd")
        if d is not None:
            P.op("sp", lambda e, d=d, i=i: e.dma_start(out=d[i * 128:(i + 1) * 128, :], in_=Wd[:]), reads=["Wd"],
                 dma="dbg8")
        P.op("pe", lambda e, i=i: e.matmul(psf(4)[:, 0:NEXP], lhsT=Ltri[:], rhs=Mall[:, i, :], start=True,
                                           stop=True), reads=["Ltri", f"Mall{i}"], writes=["ps4"])
        P.op("dve", lambda e: e.tensor_tensor(out=keyt[:], in0=psf(4)[:, 0:NEXP], in1=cnt_bc[:], op=ALU.add),
             reads=["ps4", "cnt_bc"], writes=["keyt"])
        P.op("pe", lambda e, i=i: e.matmul(psf(5)[:, 0:NEXP], lhsT=ones_bf[:], rhs=Mall[:, i, :], start=True,
                                           stop=True), reads=["ones_bf", f"Mall{i}"], writes=["ps5"])
        P.op("dve", lambda e: e.tensor_tensor(out=cnt_bc[:], in0=psf(5)[:, 0:NEXP], in1=cnt_bc[:], op=ALU.add),
             reads=["ps5", "cnt_bc"], writes=["cnt_bc"])
        P.op("dve", lambda e: e.tensor_scalar(out=jk[:], in0=keyt[:], scalar1=float(CAP), scalar2=1.0e6,
                                              op0=ALU.is_ge, op1=ALU.mult), reads=["keyt"], writes=["jk"])
        P.op("pool", lambda e: e.tensor_tensor(out=keyt[:], in0=keyt[:], in1=base_e[:], op=ALU.add),
             reads=["keyt", "base_e"], writes=["keyt"])
        P.op("pool", lambda e: e.tensor_tensor(out=keyt[:], in0=keyt[:], in1=jk[:], op=ALU.add),
             reads=["keyt", "jk"], writes=["keyt"])
        P.op("dve", lambda e: e.tensor_tensor(out=keyt[:], in0=keyt[:], in1=Mf[:], op=ALU.mult),
             reads=["keyt", "Mf"], writes=["keyt"])
        P.op("dve", lambda e: e.max(out=key8[:], in_=keyt[:]), reads=["keyt"], writes=["key8"])
        P.op("dve", lambda e, i=i: e.tensor_scalar(out=dest8[:, i, :], in0=key8[:], scalar1=-1.0, scalar2=None,
                                                   op0=ALU.add), reads=["key8"], writes=[f"dest8_{i}"])
        for k in range(8):
            P.op("dve", lambda e, i=i, k=k: e.scalar_tensor_tensor(
                out=jk[:], in0=keyt[:], scalar=key8[:, k:k + 1], in1=Wd[:], op0=ALU.is_equal, op1=ALU.mult,
                accum_out=W8[:, i, k:k + 1]), reads=["keyt", "key8", "Wd"], writes=["jk", f"W8_{i}"])
        P.op("dve", lambda e: e.tensor_scalar(out=gs8[:], in0=key8[:], scalar1=1.0e6, scalar2=None, op0=ALU.is_lt),
             reads=["key8"], writes=["gs8"])
        P.op("dve", lambda e, i=i: e.tensor_tensor(out=W8[:, i, :], in0=W8[:, i, :], in1=gs8[:], op=ALU.mult),
             reads=["gs8", f"W8_{i}"], writes=[f"W8_{i}"])
        P.op("sp", lambda e, i=i, b=b: e.dma_start(out=h2_d[i * 128:(i + 1) * 128, :], in_=h2b[b][:]),
             reads=[f"h2b{b}"], dma=f"d_h2w{b}")
        for k in range(8):
            P.op("pool", lambda e, i=i, k=k: e.indirect_dma_start(
                out=tokidx_d[:, :], out_offset=bass.IndirectOffsetOnAxis(ap=dest8[:, i, k:k + 1], axis=0),
                in_=tokid[:, i, k, :], in_offset=None, bounds_check=P.reg(e, NEXP * CAP - 1), oob_is_err=False),
                reads=[f"dest8_{i}", "tokid"], dma="d_scat", extra=[tok_init])
    f_stageA(0)
    for i_ in range(NT):
        if i_ + 1 < NT:
            f_stageA(i_ + 1)
        f_stageB(i_)
    if "Wd" in dbg:
        out_keys.append("dbg8")
    d = dbg_out("h2T", [128, KC * S], BF16)
    if d is not None:
        P.op("sp", lambda e, d=d: e.dma_start(out=d, in_=hT[:].rearrange("p a b -> p (a b)")),
             reads=[f"hT{i}" for i in range(NT)], dma="dbg9")
        out_keys.append("dbg9")
    d = dbg_out("dest8", [128, NT * 8], I32)
    if d is not None:
        P.op("sp", lambda e, d=d: e.dma_start(out=d, in_=dest8[:].rearrange("p a b -> p (a b)")),
             reads=[f"dest8_{i}" for i in range(NT)], dma="dbg10")
        out_keys.append("dbg10")
    d = dbg_out("W8", [128, NT * 8], F32)
    if d is not None:
        P.op("sp", lambda e, d=d: e.dma_start(out=d, in_=W8[:].rearrange("p a b -> p (a b)")),
             reads=[f"W8_{i}" for i in range(NT)], dma="dbg11")
        out_keys.append("dbg11")
    P.barrier()
    P.release(mF)
    if stop_after == "F":
        return finish()


    NE_RUN = int(os.environ.get("NE_RUN", str(NEXP)))
    mE = P.mark()
    top_off = P.sb_off
    P.sb_off = cat_off
    wgu = [P.sb(f"wgu{i}", [128, 2, KC, 256], BF16) for i in range(3)]
    xgb = [P.sb(f"xg{i}", [128, 2, D], BF16) for i in range(2)]
    assert P.sb_off <= cat_end
    P.sb_off = top_off
    xgb.append(P.sb("xg2", [128, 2, D], BF16))
    wdn = [P.sb(f"wdn{i}", [128, 2, D], BF16) for i in range(3)]
    xTb = [P.sb(f"xTb{i}", [128, KC, CAP], BF16) for i in range(2)]
    sgate = [P.sb(f"sgate{i}", [128, 512], F32) for i in range(2)]
    hidT = [P.sb(f"hidT{i}", [128, 2, CAP], BF16) for i in range(2)]
    obuf = [P.sb(f"obuf{i}", [128, 2, D], BF16) for i in range(2)]

    def load_expert_w(ex):
        sl = ex % 3
        P.op("pool", lambda e, ex=ex, sl=sl: e.dma_start(
            out=wgu[sl][:, 0], in_=w_gate_d[ex].rearrange("(p k) f -> p k f", p=128)),
            writes=[f"wgu{sl}"], dma=f"d_wg{sl}")
        P.op("pool", lambda e, ex=ex, sl=sl: e.dma_start(
            out=wgu[sl][:, 1], in_=w_up_d[ex].rearrange("(p k) f -> p k f", p=128)),
            writes=[f"wgu{sl}b"], dma=f"d_wu{sl}")
        P.op("pool", lambda e, ex=ex, sl=sl: e.dma_start(
            out=wdn[sl][:], in_=w_down_d[ex].rearrange("(k p) f -> p k f", p=128)),
            writes=[f"wdn{sl}"], dma=f"d_wd{sl}")

    NIDX = 6
    idxb = [P.sb(f"idxb{i}", [128, 2, 2], I32) for i in range(NIDX)]
    for sl_ in range(3):
        P.op("pool", lambda e, sl_=sl_: e.memset(xgb[sl_][:].rearrange("p b d -> p (b d)"), 0.0),
             writes=[f"xg{sl_}_0", f"xg{sl_}_1"])

    def load_expert_idx(ex):
        s3 = ex % NIDX
        for blk in range(2):
            P.op("sp", lambda e, ex=ex, s3=s3, blk=blk: e.dma_start(
                out=idxb[s3][:, blk, :], in_=tokidx_d[ex * CAP + blk * 128: ex * CAP + (blk + 1) * 128, :]),
                writes=[f"idxb{s3}_{blk}"], dma=f"d_idx{s3}_{blk}")

    def load_expert_x(ex):
        sl = ex % 3
        s3 = ex % NIDX
        for blk in range(2):
            P.op("pool", lambda e, sl=sl, s3=s3, blk=blk: e.indirect_dma_start(
                out=xgb[sl][:, blk, :], out_offset=None, in_=h2_d[:, :],
                in_offset=bass.IndirectOffsetOnAxis(ap=idxb[s3][:, blk, 0:1], axis=0),
                bounds_check=P.reg(e, S - 1), oob_is_err=False),
                reads=[f"idxb{s3}_{blk}"], writes=[f"xg{sl}_{blk}"], dma=f"d_xg{sl}_{blk}")

    def ex_T(ex):
        sl = ex % 3
        for blk in range(2):
            fns = [lambda e, c=c, blk=blk: e.transpose(
                out=psh(blk)[:, c * 128:(c + 1) * 128],
                in_=xgb[sl][:, blk, :].rearrange("p (q k) -> p k q", k=KC)[:, c, :],
                identity=ident_bf[:]) for c in range(KC)]
            P.group("pe", fns, reads=[f"xg{sl}_{blk}", "ident_bf"], writes=[f"ps{blk}"])

    def ex_T_evac(ex):
        sl = ex % 2
        P.op("act", lambda e: e.copy(out=xTb[sl][:, :, 0:128], in_=psh(0)[:, :].rearrange("p (a b) -> p a b", b=128)),
             reads=["ps0"], writes=[f"xT{sl}_0"])
        P.op("dve", lambda e: e.tensor_copy(out=xTb[sl][:, :, 128:256],
                                            in_=psh(1)[:, :].rearrange("p (a b) -> p a b", b=128)),
             reads=["ps1"], writes=[f"xT{sl}_1"])

    def ex_GU(ex):
        sl = ex % 2
        sl3 = ex % 3
        for fc in range(4):
            bank = 2 + fc // 2
            co = (fc % 2) * 256
            fns = [lambda e, k=k, fc=fc, bank=bank, co=co: e.matmul(
                psf(bank)[:, co:co + 256], lhsT=wgu[sl3][:, fc // 2, k, (fc % 2) * 128:(fc % 2 + 1) * 128],
                rhs=xTb[sl][:, k, :],
                start=(k == 0), stop=(k == KC - 1)) for k in range(KC)]
            P.group("pe", fns, reads=[f"wgu{sl3}", f"wgu{sl3}b", f"xT{sl}_0", f"xT{sl}_1"],
                    writes=[f"ps{bank}"])

    def ex_act(ex):
        sl = ex % 2
        P.op("act", lambda e: e.activation(out=sgate[sl][:], in_=psf(2)[:, :], func=AF.Silu),
             reads=["ps2"], writes=[f"sgate{sl}"])
        P.op("dve", lambda e: e.tensor_tensor(out=hidT[sl][:].rearrange("p a b -> p (a b)"),
                                              in0=psf(3)[:, :], in1=sgate[sl][:], op=ALU.mult),
             reads=["ps3", f"sgate{sl}"], writes=[f"hidT{sl}"])

    def ex_D(ex):
        sl = ex % 2
        sl3 = ex % 3
        for blk in range(2):
            for hh in range(2):
                bank = 4 + blk * 2 + hh
                fns = [lambda e, fc=fc, blk=blk, hh=hh, bank=bank: e.matmul(
                    psf(bank)[:, :], lhsT=hidT[sl][:, fc, blk * 128:(blk + 1) * 128],
                    rhs=wdn[sl3][:, fc, hh * 512:(hh + 1) * 512], start=(fc == 0), stop=(fc == 1))
                    for fc in range(2)]
                P.group("pe", fns, reads=[f"hidT{sl}", f"wdn{sl3}"], writes=[f"ps{bank}"])

    def ex_out(ex):
        sl = ex % 2
        for blk in range(2):
            for hh in range(2):
                bank = 4 + blk * 2 + hh
                if (blk * 2 + hh) % 2 == 0:
                    P.op("act", lambda e, blk=blk, hh=hh, bank=bank: e.copy(
                        out=obuf[sl][:, blk, hh * 512:(hh + 1) * 512], in_=psf(bank)[:, :]),
                        reads=[f"ps{bank}"], writes=[f"obuf{sl}"])
                else:
                    P.op("dve", lambda e, blk=blk, hh=hh, bank=bank: e.tensor_copy(
                        out=obuf[sl][:, blk, hh * 512:(hh + 1) * 512], in_=psf(bank)[:, :]),
                        reads=[f"ps{bank}"], writes=[f"obuf{sl}"])
        s3 = ex % NIDX
        for blk in range(2):
            P.op("pool", lambda e, blk=blk: e.indirect_dma_start(
                out=o8_d[:, :], out_offset=bass.IndirectOffsetOnAxis(ap=idxb[s3][:, blk, 1:2], axis=0),
                in_=obuf[sl][:, blk, :], in_offset=None, bounds_check=P.reg(e, S * 8 - 1), oob_is_err=False),
                reads=[f"obuf{sl}", f"idxb{s3}_{blk}"], dma=f"d_ob{sl}_{blk}", extra=[o8_init_tok])

    for e0 in range(min(3, NE_RUN)):
        load_expert_idx(e0)
    for e0 in range(min(2, NE_RUN)):
        load_expert_x(e0)
        load_expert_w(e0)
    if NE_RUN > 0:
        ex_T(0)
        ex_T_evac(0)
    for ex in range(NE_RUN):
        if ex + 3 < NE_RUN:
            load_expert_idx(ex + 3)
        if ex + 2 < NE_RUN:
            load_expert_x(ex + 2)
            load_expert_w(ex + 2)
        ex_GU(ex)
        if ex + 1 < NE_RUN:
            ex_T(ex + 1)
        ex_act(ex)
        if ex + 1 < NE_RUN:
            ex_T_evac(ex + 1)
        ex_D(ex)
        ex_out(ex)
    P.barrier()
    P.release(mE)
    if stop_after == "E":
        return finish()

    top_off = P.sb_off
    P.sb_off = cat_off
    gk = [P.sb(f"gk{r}", [128, 8, D], BF16) for r in range(2)]
    assert P.sb_off <= cat_end
    P.sb_off = top_off
    wsgu = P.sb("wsgu", [128, KC, 512], BF16)
    wsd = P.sb("wsd", [128, 2, D], BF16)
    diag = [P.sb(f"diag{r}", [128, 8, 128], BF16) for r in range(2)]
    sgs = [P.sb(f"sgs{r}", [128, EFF], F32) for r in range(2)]
    hs_tok = [P.sb(f"hs_tok{r}", [128, EFF], BF16) for r in range(2)]
    hsT = [P.sb(f"hsT{r}", [128, 2, 128], BF16) for r in range(2)]
    ytmp = [P.sb(f"ytmp{r}", [128, 512], F32) for r in range(2)]
    P.op("pool", lambda e: e.dma_start(out=wsgu[:, :, 0:256], in_=ws_gate_d.rearrange("(k p) f -> p k f", p=128)),
         writes=["wsgu_a"], dma="c9")
    P.op("pool", lambda e: e.dma_start(out=wsgu[:, :, 256:512], in_=ws_up_d.rearrange("(k p) f -> p k f", p=128)),
         writes=["wsgu_b"], dma="c10")
    P.op("pool", lambda e: e.dma_start(out=wsd[:], in_=ws_down_d.rearrange("(k p) f -> p k f", p=128)),
         writes=["wsd"], dma="c11")
    def c_load(i):
        r = i % 2
        P.op("sp", lambda e: e.dma_start(
            out=gk[r][:], in_=o8_d[i * 1024:(i + 1) * 1024, :].rearrange("(p k) d -> p k d", k=8)),
            writes=[f"gk{r}"], dma=f"d_gk{r}")

    def c_diag(i):
        r = i % 2
        for k in range(8):
            if k % 2 == 0:
                P.op("dve", lambda e, k=k: e.tensor_scalar(out=diag[r][:, k, :], in0=ident_bf[:],
                                                           scalar1=W8[:, i, k:k + 1], scalar2=None, op0=ALU.mult),
                     reads=["ident_bf"], writes=[f"diag{r}_{k}"])
            else:
                P.op("act", lambda e, k=k: e.activation(out=diag[r][:, k, :], in_=ident_bf[:], func=AF.Copy,
                                                        scale=W8[:, i, k:k + 1]),
                     reads=["ident_bf"], writes=[f"diag{r}_{k}"])

    def c_shared(i):
        r = i % 2
        fns = [lambda e, k=k: e.matmul(psf(0)[:, :], lhsT=hT[:, k, i * 128:(i + 1) * 128], rhs=wsgu[:, k, :],
                                       start=(k == 0), stop=(k == KC - 1)) for k in range(KC)]
        P.group("pe", fns, reads=["wsgu_a", "wsgu_b"], writes=["ps0"])
        P.op("act", lambda e: e.activation(out=sgs[r][:], in_=psf(0)[:, 0:EFF], func=AF.Silu), reads=["ps0"],
             writes=[f"sgs{r}"])
        P.op("dve", lambda e: e.tensor_tensor(out=hs_tok[r][:], in0=psf(0)[:, EFF:2 * EFF], in1=sgs[r][:],
                                              op=ALU.mult), reads=["ps0", f"sgs{r}"], writes=[f"hs_tok{r}"])
        fns = [lambda e, c=c: e.transpose(out=psh(1)[:, c * 128:(c + 1) * 128],
                                          in_=hs_tok[r][:, c * 128:(c + 1) * 128], identity=ident_bf[:])
               for c in range(2)]
        P.group("pe", fns, reads=[f"hs_tok{r}", "ident_bf"], writes=["ps1"])
        P.op("act", lambda e: e.copy(out=hsT[r][:], in_=psh(1)[:, 0:256].rearrange("p (a b) -> p a b", b=128)),
             reads=["ps1"], writes=[f"hsT{r}"])

    def c_final(i):
        r = i % 2
        for hh in range(2):
            bank = 2 + ((2 * i + hh) % 4)
            fns = [lambda e, fc=fc, hh=hh, bank=bank: e.matmul(
                psf(bank)[:, :], lhsT=hsT[r][:, fc, :], rhs=wsd[:, fc, hh * 512:(hh + 1) * 512], start=(fc == 0),
                stop=False) for fc in range(2)]
            fns += [lambda e, k=k, hh=hh, bank=bank: e.matmul(
                psf(bank)[:, :], lhsT=diag[r][:, k, :], rhs=gk[r][:, k, hh * 512:(hh + 1) * 512], start=False,
                stop=(k == 7)) for k in range(8)]
            P.group("pe", fns, reads=[f"hsT{r}", "wsd"] + [f"diag{r}_{k}" for k in range(8)] + [f"gk{r}"],
                    writes=[f"ps{bank}"])
            yb = (2 * i + hh) % 2
            P.op("dve", lambda e, hh=hh, bank=bank, yb=yb: e.tensor_tensor(
                out=ytmp[yb][:], in0=psf(bank)[:, :], in1=bc[:, 1, hh * 512:(hh + 1) * 512], op=ALU.mult),
                reads=[f"ps{bank}", "bc"], writes=[f"ytmp{yb}"])
            P.op("pool", lambda e, hh=hh, yb=yb: e.tensor_tensor(
                out=xres[:, i, hh * 512:(hh + 1) * 512], in0=ytmp[yb][:], in1=xres[:, i, hh * 512:(hh + 1) * 512],
                op=ALU.add), reads=[f"ytmp{yb}"], writes=[f"xres{i}"])
        P.op("sp", lambda e: e.dma_start(out=y_d[i * 128:(i + 1) * 128, :], in_=xres[:, i, :]),
             reads=[f"xres{i}"], dma="d_y")

    c_load(0)
    c_shared(0)
    for i in range(NT):
        if i + 1 < NT:
            c_load(i + 1)
        c_diag(i)
        if i + 1 < NT:
            c_shared(i + 1)
        c_final(i)
    out_keys.append("d_y")

    return finish()


def build_nc_2pass(debug=(), stop_after=None):
    _LAYOUT.clear()
    build_nc(debug=(), stop_after="R")
    _LAYOUT["tbl"] = _LAYOUT["tbl_new"]
    _LAYOUT["tbl_small"] = _LAYOUT["tbl_small_new"]
    return build_nc(debug=debug, stop_after=stop_after)


def make_in_maps(inputs):
    f = lambda a: np.ascontiguousarray(np.asarray(a, dtype=np.float32))
    x = f(inputs["x"])
    c = f(inputs["c"])
    w_in = f(inputs["w_in"])[0]
    perm = np.concatenate([np.arange(64, 128), np.arange(0, 64)])
    cols = []
    rq = w_in[:, 0:512].reshape(D, 4, 128)
    rk = w_in[:, 512:1024].reshape(D, 4, 128)
    cols.append(rq.reshape(D, 512))
    cols.append(rq[:, :, perm].reshape(D, 512))
    cols.append(rk.reshape(D, 512))
    cols.append(rk[:, :, perm].reshape(D, 512))
    cols.append(w_in[:, 1024:3584])
    w_in_x = np.ascontiguousarray(np.concatenate(cols, axis=1))
    shared = {
        "w_ada": f(inputs["w_ada"])[0],
        "b_ada": f(inputs["b_ada"])[0].reshape(1, 6 * D),
        "g_mix_c": np.ascontiguousarray(f(inputs["g_mix"])[0].reshape(KC, 128).T),
        "g_ffn_c": np.ascontiguousarray(f(inputs["g_ffn"])[0].reshape(KC, 128).T),
        "g_ffn_r": f(inputs["g_ffn"])[0].reshape(1, D),
        "w_in_x": w_in_x,
        "q_gain": f(inputs["q_gain"])[0].reshape(1, 64),
        "k_gain": f(inputs["k_gain"])[0].reshape(1, 64),
        "w_out": f(inputs["w_out"])[0],
        "w_router": f(inputs["w_router"])[0],
        "router_bias": f(inputs["router_bias"])[0].reshape(1, NEXP),
        "w_gate": f(inputs["w_gate"])[0],
        "w_up": f(inputs["w_up"])[0],
        "w_down": f(inputs["w_down"])[0],
        "ws_gate": f(inputs["ws_gate"])[0],
        "ws_up": f(inputs["ws_up"])[0],
        "ws_down": f(inputs["ws_down"])[0],
    }
    maps = []
    for b in range(x.shape[0]):
        m = dict(shared)
        m["x"] = x[b]
        m["cT"] = np.ascontiguousarray(c[b].reshape(KC, 128).T)
        maps.append(m)
    return maps


_NC_CACHE = {}
_LAYOUT = {}


def kernel(**inputs):
    maps = make_in_maps(inputs)
    if "nc" not in _NC_CACHE:
        _NC_CACHE["nc"] = build_nc_2pass()[0]
    nc = _NC_CACHE["nc"]
    res = run_bass_kernel_spmd(nc, maps, core_ids=list(range(8)))
    return np.stack([np.asarray(r["y"], dtype=np.float32) for r in res.results], axis=0)
```
